# Optimizing a Trainium2 kernel written in Bass

```python
import math
import jax, jax.numpy as jnp
from jax import lax
import numpy as np

D_MODEL = 1024
BATCH = 4
SEQ = 4096
DEPTH = 1
DEC_BATCH = 2
DEC_SEQ = 8192
PAST_LEN = 128

D_RNN = 1024
LRU_BLOCKS = 4
LRU_BW = D_RNN // LRU_BLOCKS
LRU_C = 8.0
CONV_W = 4
CONV_PAD_L = (CONV_W - 1) // 2
CONV_PAD_R = CONV_W - 1 - CONV_PAD_L
N_HEADS = 8
HEAD_DIM = 64
V_DIM = 2 * HEAD_DIM
ATTN_W = N_HEADS * 2 * HEAD_DIM
Q_BLOCK = 128
NUM_BUCKETS = 32
MAX_DISTANCE = 128
PEER_HEADS = 8
N_KEYS = 128
N_EXPERTS = N_KEYS * N_KEYS
PEER_TOPK = 16
D_KEY = 256
D_HALF = D_KEY // 2
PEER_CHUNK = 128
IN_W = 2 * D_RNN + 3 * ATTN_W + 2 * D_MODEL
EPS = 1e-6

kernel_name = 'hybrid_rglru_diffattn_peer_encoder'


def rmsnorm(x, g):
    xf = x.astype(jnp.float32)
    y = xf * lax.rsqrt(jnp.mean(xf * xf, axis=-1, keepdims=True) + EPS)
    return (y * g.astype(jnp.float32)).astype(x.dtype)


def rel_bucket(rel):
    nb = NUM_BUCKETS // 2
    ret = jnp.where(rel > 0, nb, 0).astype(jnp.int32)
    n = jnp.abs(rel)
    max_exact = nb // 2
    nf = jnp.maximum(n, 1).astype(jnp.float32)
    large = max_exact + (jnp.log(nf / max_exact) / math.log(MAX_DISTANCE / max_exact) * (nb - max_exact)).astype(jnp.int32)
    large = jnp.minimum(large, nb - 1)
    return ret + jnp.where(n < max_exact, n, large)


def centred_conv(x, w, b):
    S = x.shape[1]
    xp = jnp.pad(x, ((0, 0), (CONV_PAD_L, CONV_PAD_R), (0, 0)))
    y = b + xp[:, 0:S] * w[0]
    for t in range(1, CONV_W):
        y = y + xp[:, t:t + S] * w[t]
    return y


def _lin_combine(e1, e2):
    a1, b1 = e1
    a2, b2 = e2
    return a1 * a2, a2 * b1 + b2


def rglru_dir(x, w_a, b_a, w_x, b_x, lam, reverse):
    B, S, _ = x.shape
    xb = x.reshape(B, S, LRU_BLOCKS, LRU_BW)
    r = jax.nn.sigmoid((jnp.einsum('bsnc,ncd->bsnd', xb, w_a) + b_a).astype(jnp.float32)).reshape(B, S, D_RNN)
    i = jax.nn.sigmoid((jnp.einsum('bsnc,ncd->bsnd', xb, w_x) + b_x).astype(jnp.float32)).reshape(B, S, D_RNN)
    log_a = -LRU_C * r * jax.nn.softplus(-lam.astype(jnp.float32))
    a = jnp.exp(log_a)
    mult = jnp.sqrt(-jnp.expm1(2.0 * log_a))
    boundary = S - 1 if reverse else 0
    mult = mult.at[:, boundary].set(1.0)
    bx = mult * i * x.astype(jnp.float32)
    _, h = lax.associative_scan(_lin_combine, (a, bx), reverse=reverse, axis=1)
    return h


def diff_attention(q, k, v, lam, lam_init, subln_g, rel_bias):
    B, S = q.shape[0], q.shape[1]
    nblk = S // Q_BLOCK
    qb = q.reshape(B, nblk, Q_BLOCK, N_HEADS, 2, HEAD_DIM).transpose(1, 0, 2, 3, 4, 5)
    starts = jnp.arange(nblk, dtype=jnp.int32) * Q_BLOCK
    kpos = jnp.arange(S, dtype=jnp.int32)
    scale = HEAD_DIM ** -0.5

    def block(args):
        qblk, s0 = args
        qpos = s0 + jnp.arange(Q_BLOCK, dtype=jnp.int32)
        bucket = rel_bucket(kpos[None, :] - qpos[:, None])
        bias = jnp.transpose(rel_bias[bucket], (2, 0, 1)).astype(jnp.float32)
        logits = jnp.einsum('bqhcd,bkhcd->bhcqk', qblk, k).astype(jnp.float32) * scale + bias[None, :, None]
        p = jax.nn.softmax(logits, axis=-1)
        wgt = p[:, :, 0] - lam * p[:, :, 1]
        o = jnp.einsum('bhqk,bkhe->bqhe', wgt.astype(v.dtype), v)
        return rmsnorm(o, subln_g) * (1.0 - lam_init)

    out = lax.map(block, (qb, starts))
    return out.transpose(1, 0, 2, 3, 4).reshape(B, S, ATTN_W)


def peer(x, wq, keys, pu, pv):
    B, S, D = x.shape
    T = B * S
    xc = x.reshape(T // PEER_CHUNK, PEER_CHUNK, D)

    def chunk(xt):
        q = (xt @ wq).reshape(PEER_CHUNK, PEER_HEADS, 2, D_HALF)
        s = jnp.einsum('chpd,hpnd->chpn', q, keys).astype(jnp.float32)
        sv, si = lax.top_k(s, PEER_TOPK)
        cand_s = (sv[:, :, 0, :, None] + sv[:, :, 1, None, :]).reshape(PEER_CHUNK, PEER_HEADS, PEER_TOPK * PEER_TOPK)
        cand_i = (si[:, :, 0, :, None] * N_KEYS + si[:, :, 1, None, :]).reshape(PEER_CHUNK, PEER_HEADS, PEER_TOPK * PEER_TOPK)
        top_s, pos = lax.top_k(cand_s, PEER_TOPK)
        idx = jnp.take_along_axis(cand_i, pos, axis=-1)
        g = jax.nn.softmax(top_s, axis=-1)
        u = pu[idx]
        act = jax.nn.gelu(jnp.einsum('chkd,cd->chk', u, xt).astype(jnp.float32))
        return jnp.einsum('chk,chkd->cd', (g * act).astype(xt.dtype), pv[idx])

    return lax.map(chunk, xc).reshape(B, S, D)


def temporal_mix(u, w_in, b_gate, conv_w, conv_b, lru_wa_f, lru_ba_f, lru_wx_f, lru_bx_f, lru_lam_f,
                 lru_wa_b, lru_ba_b, lru_wx_b, lru_bx_b, lru_lam_b, lam, lam_init, subln_g, rel_bias,
                 w_rnn_out, w_attn_out, w_out):
    B, S, _ = u.shape
    proj = u @ w_in
    o1 = D_RNN
    o2 = 2 * D_RNN
    o3 = o2 + ATTN_W
    o4 = o3 + ATTN_W
    o5 = o4 + ATTN_W
    o6 = o5 + D_MODEL
    x_rnn, g_rnn, q, k, v, gl_r, gl_a = jnp.split(proj, [o1, o2, o3, o4, o5, o6], axis=-1)
    xc = centred_conv(x_rnn, conv_w, conv_b)
    h = (rglru_dir(xc, lru_wa_f, lru_ba_f, lru_wx_f, lru_bx_f, lru_lam_f, False)
         + rglru_dir(xc, lru_wa_b, lru_ba_b, lru_wx_b, lru_bx_b, lru_lam_b, True))
    y_rnn = (h * jax.nn.gelu(g_rnn.astype(jnp.float32))).astype(u.dtype) @ w_rnn_out
    q = q.reshape(B, S, N_HEADS, 2, HEAD_DIM)
    k = k.reshape(B, S, N_HEADS, 2, HEAD_DIM)
    v = v.reshape(B, S, N_HEADS, V_DIM)
    y_attn = diff_attention(q, k, v, lam, lam_init, subln_g, rel_bias) @ w_attn_out
    b_r, b_a = jnp.split(b_gate, 2)
    g_r = jax.nn.sigmoid((gl_r + b_r).astype(jnp.float32))
    g_a = jax.nn.sigmoid((gl_a + b_a).astype(jnp.float32))
    merged = (g_r * y_rnn.astype(jnp.float32) + g_a * y_attn.astype(jnp.float32)).astype(u.dtype)
    return merged @ w_out


def trunk(x, norm1_g, w_in, b_gate, conv_w, conv_b, lru_wa_f, lru_ba_f, lru_wx_f, lru_bx_f, lru_lam_f,
          lru_wa_b, lru_ba_b, lru_wx_b, lru_bx_b, lru_lam_b, lam_q1, lam_k1, lam_q2, lam_k2, subln_g,
          rel_bias, w_rnn_out, w_attn_out, w_out, norm2_g, peer_wq, peer_keys, peer_u, peer_v, final_g):
    for l in range(DEPTH):
        lam_init = 0.8 - 0.6 * math.exp(-0.3 * l)
        lam = (jnp.exp(jnp.sum(lam_q1[l].astype(jnp.float32) * lam_k1[l].astype(jnp.float32)))
               - jnp.exp(jnp.sum(lam_q2[l].astype(jnp.float32) * lam_k2[l].astype(jnp.float32))) + lam_init)
        u = rmsnorm(x, norm1_g[l])
        x = x + temporal_mix(u, w_in[l], b_gate[l], conv_w[l], conv_b[l],
                             lru_wa_f[l], lru_ba_f[l], lru_wx_f[l], lru_bx_f[l], lru_lam_f[l],
                             lru_wa_b[l], lru_ba_b[l], lru_wx_b[l], lru_bx_b[l], lru_lam_b[l],
                             lam, lam_init, subln_g[l], rel_bias, w_rnn_out[l], w_attn_out[l], w_out[l])
        x = x + peer(rmsnorm(x, norm2_g[l]), peer_wq[l], peer_keys[l], peer_u[l], peer_v[l])
    return rmsnorm(x, final_g)


def setup_inputs(seed: int = 0) -> dict:
    key = jax.random.key(seed)
    ks = jax.random.split(key, 40)
    f32 = jnp.float32

    def nrm(k, shape, scale):
        return jax.random.normal(k, shape, f32) * scale

    def lru_lambda(k):
        a0 = jax.random.uniform(k, (DEPTH, D_RNN), f32, 0.9, 0.999)
        p = a0 ** (1.0 / LRU_C)
        return jnp.log(p) - jnp.log1p(-p)

    return {
        'x_prompt': nrm(ks[0], (BATCH, SEQ, D_MODEL), 1.0),
        'x_sample': nrm(ks[1], (DEC_BATCH, DEC_SEQ, D_MODEL), 1.0),
        'norm1_g': 1.0 + nrm(ks[2], (DEPTH, D_MODEL), 0.02),
        'w_in': nrm(ks[3], (DEPTH, D_MODEL, IN_W), D_MODEL ** -0.5),
        'b_gate': nrm(ks[4], (DEPTH, 2 * D_MODEL), 0.1),
        'conv_w': nrm(ks[5], (DEPTH, CONV_W, D_RNN), CONV_W ** -0.5),
        'conv_b': nrm(ks[6], (DEPTH, D_RNN), 0.02),
        'lru_wa_f': nrm(ks[7], (DEPTH, LRU_BLOCKS, LRU_BW, LRU_BW), LRU_BW ** -0.5),
        'lru_ba_f': nrm(ks[8], (DEPTH, LRU_BLOCKS, LRU_BW), 0.1),
        'lru_wx_f': nrm(ks[9], (DEPTH, LRU_BLOCKS, LRU_BW, LRU_BW), LRU_BW ** -0.5),
        'lru_bx_f': nrm(ks[10], (DEPTH, LRU_BLOCKS, LRU_BW), 0.1),
        'lru_lam_f': lru_lambda(ks[11]),
        'lru_wa_b': nrm(ks[12], (DEPTH, LRU_BLOCKS, LRU_BW, LRU_BW), LRU_BW ** -0.5),
        'lru_ba_b': nrm(ks[13], (DEPTH, LRU_BLOCKS, LRU_BW), 0.1),
        'lru_wx_b': nrm(ks[14], (DEPTH, LRU_BLOCKS, LRU_BW, LRU_BW), LRU_BW ** -0.5),
        'lru_bx_b': nrm(ks[15], (DEPTH, LRU_BLOCKS, LRU_BW), 0.1),
        'lru_lam_b': lru_lambda(ks[16]),
        'lam_q1': nrm(ks[17], (DEPTH, HEAD_DIM), 0.1),
        'lam_k1': nrm(ks[18], (DEPTH, HEAD_DIM), 0.1),
        'lam_q2': nrm(ks[19], (DEPTH, HEAD_DIM), 0.1),
        'lam_k2': nrm(ks[20], (DEPTH, HEAD_DIM), 0.1),
        'subln_g': 1.0 + nrm(ks[21], (DEPTH, V_DIM), 0.02),
        'rel_bias': nrm(ks[22], (NUM_BUCKETS, N_HEADS), 0.5),
        'w_rnn_out': nrm(ks[23], (DEPTH, D_RNN, D_MODEL), D_RNN ** -0.5),
        'w_attn_out': nrm(ks[24], (DEPTH, ATTN_W, D_MODEL), ATTN_W ** -0.5),
        'w_out': nrm(ks[25], (DEPTH, D_MODEL, D_MODEL), D_MODEL ** -0.5),
        'norm2_g': 1.0 + nrm(ks[26], (DEPTH, D_MODEL), 0.02),
        'peer_wq': nrm(ks[27], (DEPTH, D_MODEL, PEER_HEADS * D_KEY), D_MODEL ** -0.5),
        'peer_keys': nrm(ks[28], (DEPTH, PEER_HEADS, 2, N_KEYS, D_HALF), D_HALF ** -0.5),
        'peer_u': nrm(ks[29], (DEPTH, N_EXPERTS, D_MODEL), D_MODEL ** -0.5),
        'peer_v': nrm(ks[30], (DEPTH, N_EXPERTS, D_MODEL), PEER_HEADS ** -0.5),
        'final_g': 1.0 + nrm(ks[31], (D_MODEL,), 0.02),
    }


def reference(x_prompt, x_sample, norm1_g, w_in, b_gate, conv_w, conv_b, lru_wa_f, lru_ba_f, lru_wx_f,
              lru_bx_f, lru_lam_f, lru_wa_b, lru_ba_b, lru_wx_b, lru_bx_b, lru_lam_b, lam_q1, lam_k1,
              lam_q2, lam_k2, subln_g, rel_bias, w_rnn_out, w_attn_out, w_out, norm2_g, peer_wq,
              peer_keys, peer_u, peer_v, final_g):
    y_prompt = trunk(x_prompt, norm1_g, w_in, b_gate, conv_w, conv_b, lru_wa_f, lru_ba_f, lru_wx_f,
                     lru_bx_f, lru_lam_f, lru_wa_b, lru_ba_b, lru_wx_b, lru_bx_b, lru_lam_b, lam_q1,
                     lam_k1, lam_q2, lam_k2, subln_g, rel_bias, w_rnn_out, w_attn_out, w_out, norm2_g,
                     peer_wq, peer_keys, peer_u, peer_v, final_g)
    y_sample = trunk(x_sample, norm1_g, w_in, b_gate, conv_w, conv_b, lru_wa_f, lru_ba_f, lru_wx_f,
                     lru_bx_f, lru_lam_f, lru_wa_b, lru_ba_b, lru_wx_b, lru_bx_b, lru_lam_b, lam_q1,
                     lam_k1, lam_q2, lam_k2, subln_g, rel_bias, w_rnn_out, w_attn_out, w_out, norm2_g,
                     peer_wq, peer_keys, peer_u, peer_v, final_g)
    return (y_prompt, y_sample)
```

```python
from contextlib import ExitStack
import math
import numpy as np
import concourse.bass as bass
import concourse.mybir as mybir
from concourse.bass_utils import run_bass_kernel_spmd

F32 = mybir.dt.float32
BF16 = mybir.dt.bfloat16
I32 = mybir.dt.int32
U32 = mybir.dt.uint32
AF = mybir.ActivationFunctionType
ALU = mybir.AluOpType
AX = mybir.AxisListType

D = 1024
NH = 8
EPS = 1e-6
LAM_INIT = 0.2
NEG = -30000.0


class Buf:
    __slots__ = ("name", "last_w", "readers", "dram", "sem", "cnt")

    def __init__(self, name, dram=False):
        self.name = name
        self.last_w = None
        self.readers = []
        self.dram = dram
        self.sem = None
        self.cnt = 0


class Eng:
    def __init__(self, name, handle, sem, self_sync=True):
        self.name = name
        self.h = handle
        self.sem = sem
        self.count = 0
        self.seen = {}
        self.self_sync = self_sync


class KB:
    def __init__(self, nc, es):
        self.nc = nc
        self.es = es
        self.nsem = 0
        self.pe = Eng("pe", nc.tensor, self.newsem("s_pe"), self_sync=False)
        self.dve = Eng("dve", nc.vector, self.newsem("s_dve"))
        self.act = Eng("act", nc.scalar, self.newsem("s_act"))
        self.pool = Eng("pool", nc.gpsimd, self.newsem("s_pool"))
        self.sp = Eng("sp", nc.sync, self.newsem("s_sp"))
        self.engs = [self.pe, self.dve, self.act, self.pool, self.sp]
        self.dbufs = []
        self.nwait = 0
        self.ninst = 0

    def newsem(self, name):
        self.nsem += 1
        return self.es.enter_context(self.nc.semaphore(f"{name}_{self.nsem}"))

    def sb(self, name, shape, dtype, es=None):
        return (es or self.es).enter_context(self.nc.sbuf_tensor(name, list(shape), dtype))

    def ps(self, name, shape, dtype):
        return self.es.enter_context(self.nc.psum_tensor(name, list(shape), dtype))

    def _wait(self, eng, tok):
        sem, val, src = tok
        if src is eng and not eng.self_sync:
            return
        key = id(sem)
        if eng.seen.get(key, 0) >= val:
            return
        eng.h.wait_ge(sem, val)
        eng.seen[key] = val
        self.nwait += 1

    def _deps(self, eng, reads, writes):
        for b in reads:
            if b.last_w is not None:
                self._wait(eng, b.last_w)
        for b in writes:
            if b.dram:
                continue
            if b.last_w is not None:
                self._wait(eng, b.last_w)
            for t in b.readers:
                self._wait(eng, t)

    def op(self, eng, fn, reads=(), writes=()):
        self._deps(eng, reads, writes)
        ins = fn()
        ins.then_inc(eng.sem, 1)
        eng.count += 1
        self.ninst += 1
        tok = (eng.sem, eng.count, eng)
        for b in writes:
            b.last_w = tok
            b.readers = []
        for b in reads:
            if not b.dram:
                b.readers.append(tok)
                if len(b.readers) > 48:
                    b.readers = b.readers[-48:]
        return tok

    def op_multi(self, eng, fns, reads=(), writes=()):
        self._deps(eng, reads, writes)
        ins = None
        for fn in fns:
            ins = fn()
            self.ninst += 1
        ins.then_inc(eng.sem, 1)
        eng.count += 1
        tok = (eng.sem, eng.count, eng)
        for b in writes:
            b.last_w = tok
            b.readers = []
        for b in reads:
            if not b.dram:
                b.readers.append(tok)
                if len(b.readers) > 48:
                    b.readers = b.readers[-48:]
        return tok

    def dma(self, eng, out, in_, src, dst, indirect=None, **kw):
        reads = [src] if src is not None else []
        writes = [dst] if dst is not None else []
        if indirect is not None:
            reads = reads + [indirect[1]]
        cb = dst if dst is not None else src
        if dst is not None and not dst.dram and dst.last_w is not None and dst.last_w[2] is None \
                and dst.last_w[0] is dst.sem and not dst.readers:
            saved = dst.last_w
            dst.last_w = None
            self._deps(eng, reads, writes)
            dst.last_w = saved
        else:
            self._deps(eng, reads, writes)
        if cb.sem is None:
            cb.sem = self.newsem("d_" + cb.name)
            self.dbufs.append(cb)
        if indirect is not None:
            ins = eng.h.indirect_dma_start(out=out, out_offset=None, in_=in_,
                                           in_offset=bass.IndirectOffsetOnAxis(ap=indirect[0], axis=0))
        else:
            ins = eng.h.dma_start(out=out, in_=in_, **kw)
        cb.cnt += 1
        ins.then_inc(cb.sem, 16)
        self.ninst += 1
        tok = (cb.sem, 16 * cb.cnt, None)
        if dst is not None:
            dst.last_w = tok
            if not dst.dram:
                dst.readers = []
        for b in reads:
            if not b.dram:
                b.readers.append(tok)
        return tok

    def barrier(self):
        toks = [(e.sem, e.count, None) for e in self.engs if e.count > 0]
        toks += [(b.sem, 16 * b.cnt, None) for b in self.dbufs if b.cnt > 0]
        for e in self.engs:
            for t in toks:
                if t[0] is e.sem:
                    continue
                self._wait(e, t)


class Ring:
    def __init__(self, kb, name, n, shape, dtype, es=None):
        self.items = [(kb.sb(f"{name}{i}", shape, dtype, es), Buf(f"{name}{i}")) for i in range(n)]
        self.i = 0

    def next(self):
        it = self.items[self.i % len(self.items)]
        self.i += 1
        return it


WNAMES = [
    ("norm1_g", [D]), ("w_in", [D, 7168]), ("b_gate", [2048]), ("conv_w", [4, D]), ("conv_b", [D]),
    ("lru_wa_f", [4, 256, 256]), ("lru_ba_f", [D]), ("lru_wx_f", [4, 256, 256]), ("lru_bx_f", [D]),
    ("lru_lam_f", [D]),
    ("lru_wa_b", [4, 256, 256]), ("lru_ba_b", [D]), ("lru_wx_b", [4, 256, 256]), ("lru_bx_b", [D]),
    ("lru_lam_b", [D]),
    ("lam_q1", [64]), ("lam_k1", [64]), ("lam_q2", [64]), ("lam_k2", [64]), ("subln_g", [128]),
    ("rel_bias", [32, 8]), ("w_rnn_out", [D, D]), ("w_attn_out", [D, D]), ("w_out", [D, D]),
    ("norm2_g", [D]), ("peer_wq", [D, 2048]), ("peer_keys", [16, 128, 128]),
    ("peer_u", [16384, D]), ("peer_v", [16384, D]), ("final_g", [D]),
]


def build(SO, phases="ABCDEF", debug=False):
    SF = 2 * SO
    NQC = SO // 512
    NKB = SF // 128
    OFFB = SO // 128
    assert OFFB >= 8
    TC = min(2048, SO)
    NTC = SF // TC
    nc = bass.Bass("TRN2", target_bir_lowering=False)

    def din(name, shape, dt=F32):
        return nc.dram_tensor(name, list(shape), dt, kind="ExternalInput").ap()

    def dscr(name, shape, dt):
        return nc.dram_tensor(name, list(shape), dt, kind="ExternalOutput" if debug else "Internal").ap()

    xf = din("xf", [SF, D])
    xo = din("xo", [SO, D])
    flags_d = din("flags", [128, 4])
    ident_d = din("ident", [128, 128])
    anti_d = din("antiid", [128, 128])
    ohb_d = din("ohb", [32, 512])
    W = {n: din(n, s) for n, s in WNAMES}
    y = nc.dram_tensor("y", [SO, D], F32, kind="ExternalOutput").ap()

    xrT = dscr("xrT", [D, SF], F32); b_xrT = Buf("xrT", True)
    kT = dscr("kT", [D, SF], BF16); b_kT = Buf("kT", True)
    vv = dscr("vv", [SF, D], BF16); b_vv = Buf("vv", True)
    qT = dscr("qT", [D, SO], BF16); b_qT = Buf("qT", True)
    ggT = dscr("ggT", [D, SO], F32); b_ggT = Buf("ggT", True)
    grT = dscr("grT", [D, SO], F32); b_grT = Buf("grT", True)
    gaT = dscr("gaT", [D, SO], F32); b_gaT = Buf("gaT", True)
    hf = dscr("hf", [D, SF], F32); b_hf = Buf("hf", True)
    hb = dscr("hb", [D, SF], F32); b_hb = Buf("hb", True)
    oT = dscr("oT", [D, SO], BF16); b_oT = Buf("oT", True)
    x1 = dscr("x1", [SO, D], F32); b_x1 = Buf("x1", True)
    tvd = dscr("tvd", [8, 512], F32); b_tvd = Buf("tvd", True)
    puv = dscr("puv", [16384, 2 * D], BF16); b_puv = Buf("puv", True)
    b_y = Buf("y", True)

    with ExitStack() as es:
        kb = KB(nc, es)
        pe, dve, act, pool, sp = kb.pe, kb.dve, kb.act, kb.pool, kb.sp
        V, A, P, T = nc.vector, nc.scalar, nc.gpsimd, nc.tensor

        psA = kb.ps("psA", [128, 2048], F32)
        psB = kb.ps("psB", [128, 2048], F32)
        bank = [(psA if i < 4 else psB)[:, (i % 4) * 512:(i % 4) * 512 + 512] for i in range(8)]
        b_bank = [Buf(f"bank{i}") for i in range(8)]

        def ctile(name, shape, dt=F32):
            return kb.sb(name, shape, dt), Buf(name)

        idf, b_idf = ctile("idf", [128, 128])
        idb, b_idb = ctile("idb", [128, 128], BF16)
        anf, b_anf = ctile("anf", [128, 128])
        anb, b_anb = ctile("anb", [128, 128], BF16)
        onesb, b_onesb = ctile("onesb", [128, 128], BF16)
        onesf, b_onesf = ctile("onesf", [128, 128])
        flg, b_flg = ctile("flg", [128, 8])
        epsb, b_epsb = ctile("epsb", [128, 1])
        kb.dma(sp, idf[:], ident_d, None, b_idf)
        kb.dma(sp, anf[:], anti_d, None, b_anf)
        kb.dma(sp, flg[:, 0:4], flags_d, None, b_flg)
        kb.op(dve, lambda: V.tensor_copy(idb[:], idf[:]), [b_idf], [b_idb])
        kb.op(dve, lambda: V.tensor_copy(anb[:], anf[:]), [b_anf], [b_anb])
        kb.op(dve, lambda: V.memset(onesb[:], 1.0), [], [b_onesb])
        kb.op(dve, lambda: V.memset(onesf[:], 1.0), [], [b_onesf])
        kb.op(dve, lambda: V.memset(epsb[:], EPS), [], [b_epsb])
        kb.op(dve, lambda: V.tensor_scalar(flg[:, 3:4], flg[:, 2:3], -1.0, 1.0, ALU.mult, ALU.add), [b_flg], [b_flg])
        kb.op(dve, lambda: V.tensor_scalar(flg[:, 4:5], flg[:, 2:3], NEG, None, ALU.mult), [b_flg], [b_flg])
        fA, fB, fP, vP, msk = (flg[:, i:i + 1] for i in range(5))

        def bcast_load(dst_ap, src_1d, n, b_dst):
            kb.dma(sp, dst_ap, src_1d.rearrange("(o n) -> o n", o=1).to_broadcast([128, n]), None, b_dst)

        def chan_load(dst_ap, src_1d, b_dst):
            kb.dma(sp, dst_ap, src_1d.rearrange("(t p) -> p t", p=128), None, b_dst,
                   allow_slow_non_contiguous=True)

        rr = {"evac": 0}

        def evac(out_ap, in_ap, reads, writes, force_act=False):
            rr["evac"] += 1
            if rr["evac"] % 2 and not force_act:
                kb.op(dve, lambda: V.tensor_copy(out_ap, in_ap), reads, writes)
            else:
                kb.op(act, lambda: A.copy(out_ap, in_ap), reads, writes)

        def norm_pieces(xsrc, t0, gbc, b_gbc, xring, xnring, uT, b_uT, ssr, junk, b_junk, bsel):
            st = {}

            def pA(tt):
                xt, b_xt = xring.next()
                kb.dma(sp, xt[:], xsrc[t0 + tt * 128:t0 + (tt + 1) * 128, :], None, b_xt)
                ss, b_ss = ssr.next()
                kb.op(act, lambda: A.activation(junk[:], xt[:], AF.Square, accum_out=ss[:]), [b_xt], [b_junk, b_ss])
                kb.op(act, lambda: A.activation(ss[:], ss[:], AF.Sqrt, bias=epsb[:], scale=1.0 / D), [b_ss, b_epsb], [b_ss])
                kb.op(dve, lambda: V.reciprocal(ss[:], ss[:]), [b_ss], [b_ss])
                xn, b_xn = xnring.next()
                kb.op(dve, lambda: V.scalar_tensor_tensor(xn[:], xt[:], ss[:, 0:1], gbc[:], ALU.mult, ALU.mult),
                      [b_xt, b_ss, b_gbc], [b_xn])
                st[tt] = (xn, b_xn)

            def pB(tt):
                xn, b_xn = st.pop(tt)
                for half in range(2):
                    bi = bsel[(tt * 2 + half) % len(bsel)]
                    kb.op_multi(pe, [lambda q=q: T.matmul(bank[bi][:, q * 128:(q + 1) * 128],
                                                          xn[:, (half * 4 + q) * 128:(half * 4 + q + 1) * 128],
                                                          idb[:], start=True, stop=True) for q in range(4)],
                                [b_xn, b_idb], [b_bank[bi]])
                    evac(uT[:, half * 4:half * 4 + 4, tt * 128:(tt + 1) * 128],
                         bank[bi].rearrange("p (a b) -> p a b", a=4), [b_bank[bi]], [b_uT])
            return pA, pB

        def pipelined_chunks(nchunks, xsrc, ngroups, body, gbc, b_gbc, xring, xnring, uTr, ssr, junk, b_junk):
            def prep(ch):
                uT, b_uT = uTr.next()
                return (uT, b_uT) + norm_pieces(xsrc, ch * 512, gbc, b_gbc, xring, xnring, uT, b_uT, ssr, junk, b_junk, [0, 1])
            cur = prep(0)
            for tt in range(4):
                cur[2](tt)
                cur[3](tt)
            step = max(ngroups // 4, 2)
            for ch in range(nchunks):
                nxt = prep(ch + 1) if ch + 1 < nchunks else None
                hooks = {}
                if nxt is not None:
                    for tt in range(4):
                        hooks[1 + step * tt] = (nxt[2], tt)
                        hooks[min(step - 2 + step * tt + 1, ngroups)] = (nxt[3], tt)
                cnt = {"g": 0}

                def tick():
                    cnt["g"] += 1
                    h_ = hooks.pop(cnt["g"], None)
                    if h_ is not None:
                        h_[0](h_[1])
                body(ch, cur[0], cur[1], tick)
                for g_ in sorted(hooks):
                    hooks[g_][0](hooks[g_][1])
                cur = nxt

        def convert_tables(pes):
            cvr = Ring(kb, "cv", 2, [128, 8, D], BF16, pes)
            for ti, nm in enumerate(("peer_u", "peer_v")):
                src = W[nm].rearrange("(p r) d -> p r d", p=128)
                dst = puv.rearrange("(p r) d -> p r d", p=128)
                for r0 in range(0, 128, 8):
                    cv, b_cv = cvr.next()
                    kb.dma(pool, cv[:], src[:, r0:r0 + 8, :], None, b_cv)
                    kb.dma(sp, dst[:, r0:r0 + 8, ti * D:(ti + 1) * D], cv[:], b_cv, b_puv)

        if "B" in phases:
            with ExitStack() as pes:
                gbc, b_gbc = kb.sb("gbc", [128, D], F32, pes), Buf("gbc")
                bcast_load(gbc[:], W["norm1_g"], D, b_gbc)
                xring = Ring(kb, "xt", 3, [128, D], F32, pes)
                xnring = Ring(kb, "xn", 3, [128, D], BF16, pes)
                ssr = Ring(kb, "ss", 3, [128, 1], F32, pes)
                junk, b_junk = kb.sb("junk", [128, D], BF16, pes), Buf("junk")
                uTr = Ring(kb, "uT", 2, [128, 8, 512], BF16, pes)
                bg, b_bg = kb.sb("bg", [128, 16], F32, pes), Buf("bg")
                kb.dma(sp, bg[:], W["b_gate"].rearrange("(t p) -> p t", p=128), None, b_bg, allow_slow_non_contiguous=True)
                with ExitStack() as pa:
                    WA, b_WA = kb.sb("WA", [128, 8, 3072], BF16, pa), Buf("WA")
                    for dt in range(8):
                        for g, c0 in enumerate((0, 3072, 4096)):
                            kb.dma(pool, WA[:, dt, g * 1024:(g + 1) * 1024], W["w_in"][dt * 128:(dt + 1) * 128, c0:c0 + 1024],
                                   None, b_WA)
                    sxr = Ring(kb, "sx", 2, [128, 8, 512], F32, pa)
                    skr = Ring(kb, "sk", 2, [128, 8, 512], BF16, pa)
                    svr = Ring(kb, "sv", 2, [128, 4, 1024], BF16, pa)
                    def bodyA(ch, uT, b_uT, tick):
                        t0 = ch * 512
                        sx, b_sx = sxr.next()
                        sk, b_sk = skr.next()
                        sv, b_sv = svr.next()
                        for j in range(16):
                            bi = 2 + j % 6
                            kb.op_multi(pe, [lambda dt=dt: T.matmul(bank[bi], WA[:, dt, j * 128:(j + 1) * 128], uT[:, dt, :],
                                                                    start=(dt == 0), stop=(dt == 7)) for dt in range(8)],
                                        [b_WA, b_uT], [b_bank[bi]])
                            if j < 8:
                                evac(sx[:, j, :], bank[bi], [b_bank[bi]], [b_sx])
                            else:
                                evac(sk[:, j - 8, :], bank[bi], [b_bank[bi]], [b_sk])
                            tick()
                        for tt in range(4):
                            for half in range(2):
                                bi = 2 + (tt * 2 + half) % 6
                                kb.op_multi(pe, [lambda dt=dt: T.matmul(bank[bi], uT[:, dt, tt * 128:(tt + 1) * 128],
                                                                        WA[:, dt, 2048 + half * 512:2048 + (half + 1) * 512],
                                                                        start=(dt == 0), stop=(dt == 7)) for dt in range(8)],
                                            [b_WA, b_uT], [b_bank[bi]])
                                evac(sv[:, tt, half * 512:(half + 1) * 512], bank[bi], [b_bank[bi]], [b_sv])
                                tick()
                        kb.dma(sp, xrT.rearrange("(c p) t -> p c t", p=128)[:, :, t0:t0 + 512], sx[:], b_sx, b_xrT)
                        kb.dma(sp, kT.rearrange("(c p) t -> p c t", p=128)[:, :, t0:t0 + 512], sk[:], b_sk, b_kT)
                        kb.dma(sp, vv[t0:t0 + 512, :].rearrange("(a p) e -> p a e", p=128), sv[:], b_sv, b_vv)

                    pipelined_chunks(SF // 512, xf, 24, bodyA, gbc, b_gbc, xring, xnring, uTr, ssr, junk, b_junk)
                    kb.barrier()
                with ExitStack() as pb:
                    WB, b_WB = kb.sb("WB", [128, 8, 4096], BF16, pb), Buf("WB")
                    for dt in range(8):
                        for g, c0 in enumerate((1024, 2048, 5120, 6144)):
                            kb.dma(pool, WB[:, dt, g * 1024:(g + 1) * 1024], W["w_in"][dt * 128:(dt + 1) * 128, c0:c0 + 1024],
                                   None, b_WB)
                    sgr = Ring(kb, "sg", 1, [128, 8, 512], F32, pb)
                    sqr = Ring(kb, "sq", 1, [128, 8, 512], BF16, pb)
                    srr = Ring(kb, "sr", 1, [128, 8, 512], F32, pb)
                    sar = Ring(kb, "sa", 1, [128, 8, 512], F32, pb)
                    def bodyB(ch, uT, b_uT, tick):
                        t0 = ch * 512
                        sg, b_sg = sgr.next()
                        sq, b_sq = sqr.next()
                        sr, b_sr = srr.next()
                        sa, b_sa = sar.next()
                        for j in range(32):
                            bi = 2 + j % 6
                            kb.op_multi(pe, [lambda dt=dt: T.matmul(bank[bi], WB[:, dt, j * 128:(j + 1) * 128], uT[:, dt, :],
                                                                    start=(dt == 0), stop=(dt == 7)) for dt in range(8)],
                                        [b_WB, b_uT], [b_bank[bi]])
                            jj = j % 8
                            if j < 8:
                                kb.op(act, lambda: A.activation(sg[:, jj, :], bank[bi], AF.Gelu_apprx_tanh), [b_bank[bi]], [b_sg])
                            elif j < 16:
                                kb.op(dve, lambda: V.tensor_copy(sq[:, jj, :], bank[bi]), [b_bank[bi]], [b_sq])
                            elif j < 24:
                                kb.op(act, lambda: A.activation(sr[:, jj, :], bank[bi], AF.Sigmoid, bias=bg[:, jj:jj + 1]),
                                      [b_bank[bi], b_bg], [b_sr])
                            else:
                                kb.op(act, lambda: A.activation(sa[:, jj, :], bank[bi], AF.Sigmoid, bias=bg[:, 8 + jj:9 + jj]),
                                      [b_bank[bi], b_bg], [b_sa])
                            tick()
                        for dst, b_dst, s_, b_s in ((ggT, b_ggT, sg, b_sg), (qT, b_qT, sq, b_sq), (grT, b_grT, sr, b_sr),
                                                    (gaT, b_gaT, sa, b_sa)):
                            kb.dma(sp, dst.rearrange("(c p) t -> p c t", p=128)[:, :, t0:t0 + 512], s_[:], b_s, b_dst)

                    pipelined_chunks(SO // 512, xo, 32, bodyB, gbc, b_gbc, xring, xnring, uTr, ssr, junk, b_junk)
                    kb.barrier()

        if "C" in phases:
            with ExitStack() as pes:
                cw, b_cw = kb.sb("cw", [128, 4, 8], F32, pes), Buf("cw")
                for j in range(4):
                    chan_load(cw[:, j, :], W["conv_w"][j, :], b_cw)
                cpar, b_cpar = kb.sb("cpar", [128, 7, 8], F32, pes), Buf("cpar")
                for i, nm in enumerate(("conv_b", "lru_ba_f", "lru_bx_f", "lru_ba_b", "lru_bx_b", "lru_lam_f", "lru_lam_b")):
                    chan_load(cpar[:, i, :], W[nm], b_cpar)
                kb.op(act, lambda: A.activation(cpar[:, 5:7, :], cpar[:, 5:7, :], AF.Exp, scale=-1.0), [b_cpar], [b_cpar])
                kb.op(act, lambda: A.activation(cpar[:, 5:7, :], cpar[:, 5:7, :], AF.Ln, bias=1.0), [b_cpar], [b_cpar])
                kb.op(dve, lambda: V.tensor_scalar(cpar[:, 5:7, :], cpar[:, 5:7, :], -8.0, None, ALU.mult), [b_cpar], [b_cpar])
                GW, b_GW = kb.sb("GW", [128, 4, 8, 256], BF16, pes), Buf("GW")
                for gi, nm in enumerate(("lru_wa_f", "lru_wx_f", "lru_wa_b", "lru_wx_b")):
                    for n in range(4):
                        kb.dma(pool, GW[:, gi, n * 2:n * 2 + 2, :], W[nm][n].rearrange("(c p) d -> p c d", p=128), None, b_GW)
                xrr = Ring(kb, "xr", 2, [128, 2, TC + 3], F32, pes)
                xcr = Ring(kb, "xc", 2, [128, 2, TC], F32, pes)
                xcbr = Ring(kb, "xcb", 2, [128, 2, TC], BF16, pes)
                rtr = Ring(kb, "rt", 2, [128, TC], F32, pes)
                itr = Ring(kb, "it", 2, [128, TC], F32, pes)
                atr = Ring(kb, "at", 2, [128, TC], F32, pes)
                mtr = Ring(kb, "mt", 2, [128, TC], F32, pes)
                bxr = Ring(kb, "bxt", 2, [128, TC], F32, pes)
                hring = Ring(kb, "ht", 2, [128, TC], F32, pes)
                st, b_st = kb.sb("st", [128, 2], F32, pes), Buf("st")
                units = []
                for n in range(4):
                    for dirn in range(2):
                        order = list(range(NTC)) if dirn == 0 else list(range(NTC - 1, -1, -1))
                        for k_, tc in enumerate(order):
                            units.append((n, dirn, tc, k_ == 0))
                cv = {}
                gt = {}

                def conv_part(u):
                    n, dirn, tc, first = u
                    xr, b_xr = xrr.next()
                    xc, b_xc = xcr.next()
                    xcb, b_xcb = xcbr.next()
                    cv[u] = (xc, b_xc, xcb, b_xcb)
                    t0 = tc * TC
                    lo, hi = max(t0 - 1, 0), min(t0 + TC + 2, SF)
                    if lo > t0 - 1:
                        kb.op(dve, lambda: V.memset(xr[:, :, 0:1], 0.0), [], [b_xr])
                    if hi < t0 + TC + 2:
                        kb.op(dve, lambda: V.memset(xr[:, :, TC + 1:TC + 3], 0.0), [], [b_xr])
                    kb.dma(sp, xr[:, :, lo - (t0 - 1):hi - (t0 - 1)],
                           xrT[n * 256:(n + 1) * 256, lo:hi].rearrange("(c p) t -> p c t", p=128), b_xrT, b_xr)
                    for c in range(2):
                        ct = n * 2 + c
                        kb.op(dve, lambda: V.tensor_scalar(xc[:, c, :], xr[:, c, 0:TC], cw[:, 0, ct:ct + 1],
                                                           cpar[:, 0, ct:ct + 1], ALU.mult, ALU.add),
                              [b_xr, b_cw, b_cpar], [b_xc])
                        for j in range(1, 4):
                            kb.op(dve, lambda: V.scalar_tensor_tensor(xc[:, c, :], xr[:, c, j:j + TC], cw[:, j, ct:ct + 1],
                                                                      xc[:, c, :], ALU.mult, ALU.add),
                                  [b_xr, b_cw, b_xc], [b_xc])
                        kb.op(act, lambda: A.copy(xcb[:, c, :], xc[:, c, :]), [b_xc], [b_xcb])

                def gate_part(u, o):
                    n, dirn, tc, first = u
                    xc, b_xc, xcb, b_xcb = cv[u]
                    ct = n * 2 + o
                    rt, b_rt = rtr.next()
                    it, b_it = itr.next()
                    gt[(u, o)] = (rt, b_rt, it, b_it)
                    for piece in range(TC // 512):
                        sl = slice(piece * 512, (piece + 1) * 512)
                        for gate in range(2):
                            bi = (piece * 2 + gate) % 8
                            kb.op_multi(pe, [lambda c=c: T.matmul(bank[bi], GW[:, dirn * 2 + gate, n * 2 + c, o * 128:(o + 1) * 128],
                                                                  xcb[:, c, sl], start=(c == 0), stop=(c == 1)) for c in range(2)],
                                        [b_GW, b_xcb], [b_bank[bi]])
                            dst, b_dst = (rt, b_rt) if gate == 0 else (it, b_it)
                            bcol = cpar[:, 1 + dirn * 2 + gate, ct:ct + 1]
                            kb.op(act, lambda: A.activation(dst[:, sl], bank[bi], AF.Sigmoid, bias=bcol),
                                  [b_bank[bi], b_cpar], [b_dst])

                def post_part(u, o):
                    n, dirn, tc, first = u
                    xc, b_xc, xcb, b_xcb = cv[u]
                    rt, b_rt, it, b_it = gt.pop((u, o))
                    ct = n * 2 + o
                    t0 = tc * TC
                    if first and o == 0:
                        kb.op(dve, lambda: V.memset(st[:], 0.0), [], [b_st])
                    at, b_at = atr.next()
                    mt, b_mt = mtr.next()
                    bxt, b_bxt = bxr.next()
                    kb.op(act, lambda: A.activation(at[:], rt[:], AF.Exp, scale=cpar[:, 5 + dirn, ct:ct + 1]),
                          [b_rt, b_cpar], [b_at])
                    kb.op(dve, lambda: V.scalar_tensor_tensor(mt[:], at[:], -1.0, at[:], ALU.mult, ALU.mult), [b_at], [b_mt])
                    kb.op(dve, lambda: V.tensor_scalar(mt[:], mt[:], 1.0, 1e-30, ALU.add, ALU.max), [b_mt], [b_mt])
                    kb.op(act, lambda: A.activation(mt[:], mt[:], AF.Sqrt), [b_mt], [b_mt])
                    if dirn == 0 and tc == 0:
                        kb.op(dve, lambda: V.memset(mt[:, 0:1], 1.0), [], [b_mt])
                    if dirn == 1 and tc == NTC - 1:
                        kb.op(dve, lambda: V.memset(mt[:, TC - 1:TC], 1.0), [], [b_mt])
                    if dirn == 1 and tc == NTC // 2 - 1:
                        kb.op(dve, lambda: V.tensor_scalar(mt[:, TC - 1:TC], mt[:, TC - 1:TC], vP, fP, ALU.mult, ALU.add),
                              [b_mt, b_flg], [b_mt])
                    kb.op(dve, lambda: V.tensor_tensor(bxt[:], it[:], xc[:, o, :], ALU.mult), [b_it, b_xc], [b_bxt])
                    kb.op(dve, lambda: V.tensor_tensor(bxt[:], bxt[:], mt[:], ALU.mult), [b_bxt, b_mt], [b_bxt])
                    if dirn == 1 and tc >= NTC // 2:
                        kb.op(dve, lambda: V.tensor_scalar(bxt[:], bxt[:], vP, None, ALU.mult), [b_bxt, b_flg], [b_bxt])
                    ht, b_ht = hring.next()
                    if dirn == 0:
                        kb.op(dve, lambda: V.tensor_tensor_scan(ht[:], at[:], bxt[:], st[:, o:o + 1], ALU.mult, ALU.add),
                              [b_at, b_bxt, b_st], [b_ht])
                        kb.op(dve, lambda: V.tensor_copy(st[:, o:o + 1], ht[:, TC - 1:TC]), [b_ht], [b_st])
                    else:
                        kb.op(dve, lambda: V.tensor_tensor_scan(ht[:, ::-1], at[:, ::-1], bxt[:, ::-1], st[:, o:o + 1],
                                                                ALU.mult, ALU.add), [b_at, b_bxt, b_st], [b_ht])
                        kb.op(dve, lambda: V.tensor_copy(st[:, o:o + 1], ht[:, 0:1]), [b_ht], [b_st])
                    dst, b_dst = (hf, b_hf) if dirn == 0 else (hb, b_hb)
                    kb.dma(sp, dst[ct * 128:(ct + 1) * 128, t0:t0 + TC], ht[:], b_ht, b_dst)

                conv_part(units[0])
                for i_, u in enumerate(units):
                    gate_part(u, 0)
                    gate_part(u, 1)
                    if i_ + 1 < len(units):
                        conv_part(units[i_ + 1])
                    post_part(u, 0)
                    post_part(u, 1)
                    cv.pop(u)
                kb.barrier()

        if "D" in phases:
            with ExitStack() as pes:
                lq, b_lq = kb.sb("lq", [128, 4, 64], F32, pes), Buf("lq")
                for i, nm in enumerate(("lam_q1", "lam_k1", "lam_q2", "lam_k2")):
                    bcast_load(lq[:, i, :], W[nm], 64, b_lq)
                lj, b_lj = kb.sb("lj", [128, 64], F32, pes), Buf("lj")
                lam, b_lam = kb.sb("lam", [128, 4], F32, pes), Buf("lam")
                for i in range(2):
                    kb.op(dve, lambda: V.scalar_tensor_tensor(lj[:], lq[:, 2 * i, :], 1.0, lq[:, 2 * i + 1, :], ALU.mult, ALU.mult,
                                                              accum_out=lam[:, i:i + 1]), [b_lq], [b_lj, b_lam])
                kb.op(act, lambda: A.activation(lam[:, 0:2], lam[:, 0:2], AF.Exp), [b_lam], [b_lam])
                kb.op(dve, lambda: V.tensor_tensor(lam[:, 2:3], lam[:, 0:1], lam[:, 1:2], ALU.subtract), [b_lam], [b_lam])
                kb.op(dve, lambda: V.tensor_scalar(lam[:, 3:4], lam[:, 2:3], -1.0, -LAM_INIT, ALU.mult, ALU.add), [b_lam], [b_lam])
                nlam = lam[:, 3:4]
                sg, b_sg = kb.sb("sgn", [128, 1], F32, pes), Buf("sgn")
                kb.dma(sp, sg[:], W["subln_g"].rearrange("(p o) -> p o", o=1), None, b_sg)
                kb.op(dve, lambda: V.tensor_scalar(sg[:], sg[:], 1.0 - LAM_INIT, None, ALU.mult), [b_sg], [b_sg])
                rb, b_rb = kb.sb("rb", [32, 8], F32, pes), Buf("rb")
                kb.dma(sp, rb[:], W["rel_bias"], None, b_rb)
                ohb, b_ohb = kb.sb("ohbs", [32, 512], F32, pes), Buf("ohbs")
                kb.dma(sp, ohb[:], ohb_d, None, b_ohb)
                kb.op(pe, lambda: T.matmul(bank[0][0:8, :], rb[:], ohb[:], start=True, stop=True), [b_rb, b_ohb], [b_bank[0]])
                tvs, b_tvs = kb.sb("tvs", [8, 512], F32, pes), Buf("tvs")
                kb.op(dve, lambda: V.tensor_copy(tvs[:], bank[0][0:8, :]), [b_bank[0]], [b_tvs])
                kb.dma(sp, tvd, tvs[:], b_tvs, b_tvd)
                Ef, b_Ef = kb.sb("Ef", [128, 8, 3, 128], F32, pes), Buf("Ef")
                for h in range(NH):
                    for mi, m in enumerate((-1, 0, 1)):
                        src = bass.AP(tvd.tensor, h * 512 + 256 - 128 * m - 127, [[1, 128], [1, 128]])
                        kb.dma(sp, Ef[:, h, mi, :], src, b_tvd, b_Ef)
                cb, b_cb = kb.sb("cb", [128, 2, 8], F32, pes), Buf("cb")
                bcast_load(cb[:, 0, :], W["rel_bias"][15, :], 8, b_cb)
                bcast_load(cb[:, 1, :], W["rel_bias"][31, :], 8, b_cb)
                bc, b_bc = kb.sb("bc", [128, 8, 8], F32, pes), Buf("bc")
                kb.op(dve, lambda: V.memset(bc[:], 0.0), [], [b_bc])
                kb.op(dve, lambda: V.tensor_copy(bc[:, :, 0], cb[:, 0, :]), [b_cb], [b_bc])
                kb.op(dve, lambda: V.tensor_copy(bc[:, :, 2], cb[:, 1, :]), [b_cb], [b_bc])
                kb.op(dve, lambda: V.tensor_scalar(bc[:, :, 1], cb[:, 1, :], fA, None, ALU.mult), [b_cb, b_flg], [b_bc])
                kb.op(dve, lambda: V.scalar_tensor_tensor(bc[:, :, 1], cb[:, 0, :], fB, bc[:, :, 1], ALU.mult, ALU.add),
                      [b_cb, b_flg, b_bc], [b_bc])
                kb.op(dve, lambda: V.tensor_scalar(bc[:, :, 3], bc[:, :, 1], msk, None, ALU.add), [b_bc, b_flg], [b_bc])
                kb.op(dve, lambda: V.tensor_scalar(bc[:, :, 4], bc[:, :, 2], msk, None, ALU.add), [b_bc, b_flg], [b_bc])
                kb.op(dve, lambda: V.tensor_scalar(bc[:, :, 6], bc[:, :, 5], msk, None, ALU.add), [b_bc, b_flg], [b_bc])
                Gf, b_Gf = kb.sb("Gf", [128, 9, 128], F32, pes), Buf("Gf")
                Gh, b_Gh = kb.sb("Gh", [128, 9, 128], BF16, pes), Buf("Gh")
                Gl, b_Gl = kb.sb("Gl", [128, 9, 128], BF16, pes), Buf("Gl")
                tmpc, b_tmpc = kb.sb("tmpc", [128, 2], F32, pes), Buf("tmpc")

                def gidx(m, side):
                    if side == 0:
                        return 6 if m <= -2 else (7 if m >= 2 else m + 1)
                    return 7 if m <= -2 else (8 if m >= 2 else 3 + m + 1)

                kr = Ring(kb, "kTh", 2, [128, SF], BF16, pes)
                vr = Ring(kb, "vh", 2, [128, NKB, 128], BF16, pes)
                qr = Ring(kb, "qTh", 2, [128, SO], BF16, pes)
                rz, b_rz = kb.sb("rz", [128, 2, 512], F32, pes), Buf("rz")
                dd, b_dd = kb.sb("dd", [128, 2, 512], F32, pes), Buf("dd")
                sqt, b_sqt = kb.sb("sqt", [128, 512], F32, pes), Buf("sqt")
                rstd, b_rstd = kb.sb("rstd", [128, 512], F32, pes), Buf("rstd")
                orr = Ring(kb, "ot", 2, [128, 512], BF16, pes)
                ptr = Ring(kb, "ptw", 4, [128, 1024], BF16, pes)
                OB = [4, 5]
                ZB = [6, 7]
                XB = 0

                def load_head(h):
                    kTh, b_kTh = kr.next()
                    vh, b_vh = vr.next()
                    qTh, b_qTh = qr.next()
                    kb.dma(sp, kTh[:], kT[h * 128:(h + 1) * 128, :], b_kT, b_kTh)
                    kb.dma(sp, vh[:], vv[:, h * 128:(h + 1) * 128].rearrange("(a p) e -> p a e", p=128), b_vv, b_vh)
                    kb.dma(sp, qTh[:], qT[h * 128:(h + 1) * 128, :], b_qT, b_qTh)
                    return (kTh, b_kTh, vh, b_vh, qTh, b_qTh)

                pend = {"a": None, "b": None}
                nxt = load_head(0)
                convert_tables(pes)
                for h in range(NH):
                    kb.op(dve, lambda: V.tensor_scalar(tmpc[:, 0:1], cb[:, 0, h:h + 1], fB, None, ALU.mult), [b_cb, b_flg], [b_tmpc])
                    kb.op(dve, lambda: V.tensor_scalar(tmpc[:, 1:2], cb[:, 1, h:h + 1], fA, None, ALU.mult), [b_cb, b_flg], [b_tmpc])
                    for mi in range(3):
                        kb.op(dve, lambda: V.tensor_scalar(Gf[:, mi, :], Ef[:, h, mi, :], fA, tmpc[:, 0:1], ALU.mult, ALU.add),
                              [b_Ef, b_flg, b_tmpc], [b_Gf])
                        kb.op(dve, lambda: V.tensor_scalar(Gf[:, 3 + mi, :], Ef[:, h, mi, :], fB, tmpc[:, 1:2], ALU.mult, ALU.add),
                              [b_Ef, b_flg, b_tmpc], [b_Gf])
                    for ci in range(3):
                        kb.op(dve, lambda: V.tensor_scalar(Gf[:, 6 + ci, :], onesf[:], bc[:, h, ci:ci + 1], None, ALU.mult),
                              [b_onesf, b_bc], [b_Gf])
                    kb.op(dve, lambda: V.tensor_scalar(Gf[:], Gf[:], 8.0, None, ALU.mult), [b_Gf], [b_Gf])
                    kb.op(dve, lambda: V.tensor_copy(Gh[:], Gf[:]), [b_Gf], [b_Gh])
                    kb.op(dve, lambda: V.tensor_tensor(Gl[:], Gf[:], Gh[:], ALU.subtract), [b_Gf, b_Gh], [b_Gl])
                    kTh, b_kTh, vh, b_vh, qTh, b_qTh = nxt
                    if h + 1 < NH:
                        nxt = load_head(h + 1)
                    for qc in range(NQC):
                        pts = {}

                        def stage1(kbk):
                            dA = kbk - 4 * qc
                            dB = dA - OFFB
                            nearA = -1 <= dA <= 4
                            nearB = -1 <= dB <= 4
                            s_ = kbk % 2
                            fns = []
                            for c in range(2):
                                ps = slice(c * 64, (c + 1) * 64)
                                bi = 2 * s_ + c
                                fns.append(lambda ps=ps, bi=bi: T.matmul(bank[bi], kTh[ps, kbk * 128:(kbk + 1) * 128],
                                                                         qTh[ps, qc * 512:(qc + 1) * 512],
                                                                         start=True, stop=not (nearA or nearB)))
                            reads = [b_kTh, b_qTh]
                            if nearA or nearB:
                                reads += [b_anb, b_Gh, b_Gl]
                                for c in range(2):
                                    bi = 2 * s_ + c
                                    for j in range(4):
                                        gi = gidx(dA - j, 0) if nearA else gidx(dB - j, 1)
                                        fns.append(lambda bi=bi, j=j, gi=gi: T.matmul(bank[bi][:, j * 128:(j + 1) * 128], anb[:], Gh[:, gi, :],
                                                                                      start=False, stop=False))
                                        fns.append(lambda bi=bi, j=j, gi=gi: T.matmul(bank[bi][:, j * 128:(j + 1) * 128], anb[:], Gl[:, gi, :],
                                                                                      start=False, stop=(j == 3)))
                            kb.op_multi(pe, fns, reads, [b_bank[2 * s_], b_bank[2 * s_ + 1]])

                        def stage2(kbk):
                            dA = kbk - 4 * qc
                            dB = dA - OFFB
                            m2 = kbk >= OFFB
                            if (-1 <= dA <= 4) or (-1 <= dB <= 4):
                                col = 6 if m2 else 5
                            else:
                                col = 0 if dA < 5 else (1 if dB < 5 else 2)
                                if m2:
                                    col = {1: 3, 2: 4}[col]
                            s_ = kbk % 2
                            pt, b_pt = ptr.next()
                            pts[kbk] = (pt, b_pt)
                            kb.op(act, lambda: A.activation(pt[:], psA[:, s_ * 1024:(s_ + 1) * 1024], AF.Exp,
                                                            bias=bc[:, h, col:col + 1], scale=0.125),
                                  [b_bank[2 * s_], b_bank[2 * s_ + 1], b_bc], [b_pt])

                        def stage3(kbk):
                            pt, b_pt = pts.pop(kbk)
                            fns = []
                            for c in range(2):
                                fns.append(lambda c=c: T.matmul(bank[OB[c]], vh[:, kbk, :], pt[:, c * 512:(c + 1) * 512],
                                                                start=(kbk == 0), stop=(kbk == NKB - 1)))
                                fns.append(lambda c=c: T.matmul(bank[ZB[c]], onesb[:], pt[:, c * 512:(c + 1) * 512],
                                                                start=(kbk == 0), stop=(kbk == NKB - 1)))
                            kb.op_multi(pe, fns, [b_vh, b_onesb, b_pt], [b_bank[OB[0]], b_bank[OB[1]], b_bank[ZB[0]], b_bank[ZB[1]]])

                        stage1(0)
                        stage1(1)
                        for kbk in range(NKB):
                            stage2(kbk)
                            if kbk == 3 and pend["a"] is not None:
                                pend["a"](2 * (kbk % 2))
                                pend["a"] = None
                            if kbk == 7 and pend["b"] is not None:
                                pend["b"]()
                                pend["b"] = None
                            if kbk + 2 < NKB:
                                stage1(kbk + 2)
                            stage3(kbk)
                        for c in range(2):
                            kb.op(dve, lambda: V.reciprocal(rz[:, c, :], bank[ZB[c]]), [b_bank[ZB[c]]], [b_rz])
                            kb.op(dve, lambda: V.tensor_tensor(dd[:, c, :], bank[OB[c]], rz[:, c, :], ALU.mult),
                                  [b_bank[OB[c]], b_rz], [b_dd])
                        kb.op(dve, lambda: V.scalar_tensor_tensor(dd[:, 0, :], dd[:, 1, :], nlam, dd[:, 0, :], ALU.mult, ALU.add),
                              [b_dd, b_lam], [b_dd])
                        def part2a(xb):
                            kb.op(pool, lambda: P.tensor_tensor(sqt[:], dd[:, 0, :], dd[:, 0, :], ALU.mult), [b_dd], [b_sqt])
                            kb.op(pe, lambda: T.matmul(bank[xb], onesf[:], sqt[:], start=True, stop=True), [b_onesf, b_sqt], [b_bank[xb]])
                            kb.op(dve, lambda: V.tensor_scalar(rstd[:], bank[xb], 1.0 / 128, EPS, ALU.mult, ALU.add), [b_bank[xb]], [b_rstd])

                        def part2b(h=h, qc=qc):
                            kb.op(act, lambda: A.activation(rstd[:], rstd[:], AF.Ln), [b_rstd], [b_rstd])
                            kb.op(act, lambda: A.activation(rstd[:], rstd[:], AF.Exp, scale=-0.5), [b_rstd], [b_rstd])
                            ot, b_ot = orr.next()
                            kb.op(dve, lambda: V.scalar_tensor_tensor(ot[:], dd[:, 0, :], sg[:, 0:1], rstd[:], ALU.mult, ALU.mult),
                                  [b_dd, b_sg, b_rstd], [b_ot])
                            kb.dma(sp, oT[h * 128:(h + 1) * 128, qc * 512:(qc + 1) * 512], ot[:], b_ot, b_oT)

                        pend["a"], pend["b"] = part2a, part2b
                pend["a"](0)
                pend["b"]()
                kb.barrier()

        if "E" in phases:
            with ExitStack() as pes:
                WO, b_WO = kb.sb("WO", [128, 3, 8, D], BF16, pes), Buf("WO")
                for i, nm in enumerate(("w_rnn_out", "w_attn_out", "w_out")):
                    for dt in range(8):
                        kb.dma(pool, WO[:, i, dt, :], W[nm][dt * 128:(dt + 1) * 128, :], None, b_WO)
                h4 = [(kb.sb(f"h4_{i}", [128, 8, 512], F32, pes), Buf(f"h4_{i}")) for i in range(4)]
                gg, b_gg = kb.sb("gg", [128, 8, 512], F32, pes), Buf("gg")
                hg, b_hg = kb.sb("hg", [128, 8, 512], BF16, pes), Buf("hg")
                ob, b_ob = kb.sb("ob", [128, 8, 512], BF16, pes), Buf("ob")
                gr, b_gr = kb.sb("gr", [128, 8, 512], F32, pes), Buf("gr")
                ga, b_ga = kb.sb("ga", [128, 8, 512], F32, pes), Buf("ga")
                m1, b_m1 = kb.sb("m1", [128, 512], F32, pes), Buf("m1")
                m2t, b_m2t = kb.sb("m2t", [128, 512], F32, pes), Buf("m2t")
                mg, b_mg = kb.sb("mg", [128, 8, 512], BF16, pes), Buf("mg")
                xor_ = Ring(kb, "xo", 2, [128, D], F32, pes)
                x1r = Ring(kb, "x1t", 2, [128, D], F32, pes)
                hv = lambda a: a.rearrange("(c p) t -> p c t", p=128)
                for ch in range(SO // 512):
                    t0 = ch * 512
                    for i, (src, b_src, off) in enumerate(((hf, b_hf, 0), (hb, b_hb, 0), (hf, b_hf, SO), (hb, b_hb, SO))):
                        kb.dma(sp, h4[i][0][:], hv(src)[:, :, off + t0:off + t0 + 512], b_src, h4[i][1])
                    kb.dma(sp, gg[:], hv(ggT)[:, :, t0:t0 + 512], b_ggT, b_gg)
                    kb.dma(sp, ob[:], hv(oT)[:, :, t0:t0 + 512], b_oT, b_ob)
                    kb.dma(sp, gr[:], hv(grT)[:, :, t0:t0 + 512], b_grT, b_gr)
                    kb.dma(sp, ga[:], hv(gaT)[:, :, t0:t0 + 512], b_gaT, b_ga)
                    a0, a1, a2, a3 = (t[0] for t in h4)
                    kb.op(dve, lambda: V.tensor_tensor(a0[:], a0[:], a1[:], ALU.add), [h4[0][1], h4[1][1]], [h4[0][1]])
                    kb.op(pool, lambda: P.tensor_tensor(a2[:], a2[:], a3[:], ALU.add), [h4[2][1], h4[3][1]], [h4[2][1]])
                    kb.op(dve, lambda: V.tensor_scalar(a0[:], a0[:], fA, None, ALU.mult), [h4[0][1], b_flg], [h4[0][1]])
                    kb.op(dve, lambda: V.scalar_tensor_tensor(a0[:], a2[:], fB, a0[:], ALU.mult, ALU.add),
                          [h4[0][1], h4[2][1], b_flg], [h4[0][1]])
                    kb.op(dve, lambda: V.tensor_tensor(hg[:], a0[:], gg[:], ALU.mult), [h4[0][1], b_gg], [b_hg])
                    for j in range(8):
                        b1, b2 = (2 * j) % 8, (2 * j + 1) % 8
                        for dt in range(8):
                            kb.op(pe, lambda: T.matmul(bank[b1], WO[:, 0, dt, j * 128:(j + 1) * 128], hg[:, dt, :],
                                                       start=(dt == 0), stop=(dt == 7)), [b_WO, b_hg], [b_bank[b1]])
                        for dt in range(8):
                            kb.op(pe, lambda: T.matmul(bank[b2], WO[:, 1, dt, j * 128:(j + 1) * 128], ob[:, dt, :],
                                                       start=(dt == 0), stop=(dt == 7)), [b_WO, b_ob], [b_bank[b2]])
                        kb.op(dve, lambda: V.tensor_tensor(m1[:], bank[b1], gr[:, j, :], ALU.mult), [b_bank[b1], b_gr], [b_m1])
                        kb.op(dve, lambda: V.tensor_tensor(m2t[:], bank[b2], ga[:, j, :], ALU.mult), [b_bank[b2], b_ga], [b_m2t])
                        kb.op(pool, lambda: P.tensor_tensor(mg[:, j, :], m1[:], m2t[:], ALU.add), [b_m1, b_m2t], [b_mg])
                    for tt in range(4):
                        xt, b_xt = xor_.next()
                        kb.dma(sp, xt[:], xo[t0 + tt * 128:t0 + (tt + 1) * 128, :], None, b_xt)
                        x1t, b_x1t = x1r.next()
                        for half in range(2):
                            bi = (tt * 2 + half) % 8
                            for dt in range(8):
                                kb.op(pe, lambda: T.matmul(bank[bi], mg[:, dt, tt * 128:(tt + 1) * 128],
                                                           WO[:, 2, dt, half * 512:(half + 1) * 512],
                                                           start=(dt == 0), stop=(dt == 7)), [b_WO, b_mg], [b_bank[bi]])
                            kb.op(dve, lambda: V.tensor_tensor(x1t[:, half * 512:(half + 1) * 512], bank[bi],
                                                               xt[:, half * 512:(half + 1) * 512], ALU.add),
                                  [b_bank[bi], b_xt], [b_x1t])
                        kb.dma(sp, x1[t0 + tt * 128:t0 + (tt + 1) * 128, :], x1t[:], b_x1t, b_x1)
                kb.barrier()

        if "F" in phases:
            with ExitStack() as pes:
                if "D" not in phases:
                    convert_tables(pes)
                g2, b_g2 = kb.sb("g2", [128, D], F32, pes), Buf("g2")
                bcast_load(g2[:], W["norm2_g"], D, b_g2)
                gf, b_gf = kb.sb("gfin", [128, D], F32, pes), Buf("gfin")
                bcast_load(gf[:], W["final_g"], D, b_gf)
                WQ, b_WQ = kb.sb("WQ", [128, 8, 2048], BF16, pes), Buf("WQ")
                for dt in range(8):
                    for g in range(2):
                        kb.dma(pool, WQ[:, dt, g * 1024:(g + 1) * 1024], W["peer_wq"][dt * 128:(dt + 1) * 128, g * 1024:(g + 1) * 1024],
                               None, b_WQ)
                kf, b_kf = kb.sb("kf", [128, 16, 128], BF16, pes), Buf("kf")
                kb.dma(pool, kf[:], W["peer_keys"].rearrange("a n d -> n a d"), None, b_kf)
                kTt, b_kTt = kb.sb("kTt", [128, 16, 128], BF16, pes), Buf("kTt")
                for hp in range(16):
                    bi = hp // 4
                    kb.op(pe, lambda: T.matmul(bank[bi][:, (hp % 4) * 128:(hp % 4 + 1) * 128], kf[:, hp, :], idb[:],
                                               start=True, stop=True), [b_kf, b_idb], [b_bank[bi]])
                    if hp % 4 == 3:
                        evac(kTt[:, bi * 4:bi * 4 + 4, :], bank[bi].rearrange("p (a b) -> p a b", a=4), [b_bank[bi]], [b_kTt])
                io16, b_io16 = kb.sb("io16", [128, 16], F32, pes), Buf("io16")
                kb.op(pool, lambda: P.iota(io16[:], [[1, 16]], base=0, channel_multiplier=0, allow_small_or_imprecise_dtypes=True),
                      [], [b_io16])
                thr16 = kb.sb("thr16", [128, 16], F32, pes)
                kb.op(dve, lambda: V.tensor_scalar(thr16[:], io16[:], 16.0, 16.0, ALU.mult, ALU.add), [b_io16], [b_io16])
                X1 = [(kb.sb(f"fx1_{i}", [128, D], F32, pes), Buf(f"fx1_{i}")) for i in range(2)]
                XN = [(kb.sb(f"fxn_{i}", [128, D], F32, pes), Buf(f"fxn_{i}")) for i in range(2)]
                IDX = [(kb.sb(f"fidx_{i}", [128, 128], U32, pes), Buf(f"fidx_{i}")) for i in range(2)]
                GW = [(kb.sb(f"fgw_{i}", [128, 8, 16], F32, pes), Buf(f"fgw_{i}")) for i in range(2)]
                junkA, b_junkA = kb.sb("fjunkA", [128, D], BF16, pes), Buf("fjunkA")
                ssA, b_ssA = kb.sb("fssA", [128, 2], F32, pes), Buf("fssA")
                junk, b_junk = kb.sb("fjunk", [128, D], F32, pes), Buf("fjunk")
                ss, b_ss = kb.sb("fss", [128, 2], F32, pes), Buf("fss")
                xnb, b_xnb = kb.sb("fxnb", [128, D], BF16, pes), Buf("fxnb")
                xT, b_xT = kb.sb("fxT", [128, 8, 128], BF16, pes), Buf("fxT")
                qTt, b_qTt = kb.sb("fqT", [128, 16, 128], BF16, pes), Buf("fqT")
                sc, b_sc = kb.sb("fsc", [128, 16, 128], F32, pes), Buf("fsc")
                sc2, b_sc2 = kb.sb("fsc2", [128, 16, 128], F32, pes), Buf("fsc2")
                svl, b_svl = kb.sb("fsv", [128, 16, 16], F32, pes), Buf("fsv")
                siu, b_siu = kb.sb("fsiu", [128, 16, 16], U32, pes), Buf("fsiu")
                sif, b_sif = kb.sb("fsif", [128, 16, 16], F32, pes), Buf("fsif")
                cand, b_cand = kb.sb("fcand", [128, 8, 256], F32, pes), Buf("fcand")
                cand2, b_cand2 = kb.sb("fcand2", [128, 8, 256], F32, pes), Buf("fcand2")
                ts, b_ts = kb.sb("fts", [128, 8, 16], F32, pes), Buf("fts")
                pu32, b_pu32 = kb.sb("fpu", [128, 8, 16], U32, pes), Buf("fpu")
                posf, b_posf = kb.sb("fposf", [128, 8, 16], F32, pes), Buf("fposf")
                pj, b_pj = kb.sb("fpj", [128, 8, 16], F32, pes), Buf("fpj")
                pi_, b_pi = kb.sb("fpi", [128, 8, 16], F32, pes), Buf("fpi")
                eq, b_eq = kb.sb("feq", [128, 8, 16, 16], F32, pes), Buf("feq")
                I1, b_I1 = kb.sb("fI1", [128, 8, 16], F32, pes), Buf("fI1")
                I2, b_I2 = kb.sb("fI2", [128, 8, 16], F32, pes), Buf("fI2")
                nmx, b_nmx = kb.sb("fnmx", [128, 8], F32, pes), Buf("fnmx")
                zs, b_zs = kb.sb("fzs", [128, 8], F32, pes), Buf("fzs")
                actt, b_actt = kb.sb("fact", [128, 128], F32, pes), Buf("fact")
                wgt, b_wgt = kb.sb("fwgt", [128, 128], F32, pes), Buf("fwgt")
                acc, b_acc = kb.sb("facc", [128, D], F32, pes), Buf("facc")
                ur = Ring(kb, "fU", 16, [128, 2 * D], BF16, pes)
                dgr = Ring(kb, "fdg", 4, [128, 128], BF16, pes)
                yt, b_yt = kb.sb("fyt", [128, D], F32, pes), Buf("fyt")
                BW = [Buf(f"wgt{i}") for i in range(32)]
                BA = [Buf(f"act{i}") for i in range(32)]
                def f_a0(tt):
                        x1t, b_x1t = X1[tt % 2]
                        xn, b_xn = XN[tt % 2]
                        idxu, b_idxu = IDX[tt % 2]
                        gw, b_gw = GW[tt % 2]
                        gwf = gw.rearrange("p h k -> p (h k)")
                        r0 = tt * 128
                        sv4 = svl.rearrange("p (h two) k -> p h two k", two=2)
                        si4 = sif.rearrange("p (h two) k -> p h two k", two=2)
                        c4 = cand.rearrange("p h (i j) -> p h i j", i=16)
                        B4 = [128, 8, 16, 16]
                        r0 = tt * 128
                        kb.dma(sp, x1t[:], x1[r0:r0 + 128, :], b_x1, b_x1t)
                        kb.op(act, lambda: A.activation(junkA[:], x1t[:], AF.Square, accum_out=ssA[:, 0:1]), [b_x1t], [b_junkA, b_ssA])
                        kb.op(act, lambda: A.activation(ssA[:, 0:1], ssA[:, 0:1], AF.Sqrt, bias=epsb[:], scale=1.0 / D), [b_ssA, b_epsb], [b_ssA])
                def f_a(tt):
                        x1t, b_x1t = X1[tt % 2]
                        xn, b_xn = XN[tt % 2]
                        idxu, b_idxu = IDX[tt % 2]
                        gw, b_gw = GW[tt % 2]
                        gwf = gw.rearrange("p h k -> p (h k)")
                        r0 = tt * 128
                        sv4 = svl.rearrange("p (h two) k -> p h two k", two=2)
                        si4 = sif.rearrange("p (h two) k -> p h two k", two=2)
                        c4 = cand.rearrange("p h (i j) -> p h i j", i=16)
                        B4 = [128, 8, 16, 16]
                        r0 = tt * 128
                        kb.op(dve, lambda: V.reciprocal(ssA[:, 0:1], ssA[:, 0:1]), [b_ssA], [b_ssA])
                        kb.op(dve, lambda: V.scalar_tensor_tensor(xn[:], x1t[:], ssA[:, 0:1], g2[:], ALU.mult, ALU.mult),
                              [b_x1t, b_ssA, b_g2], [b_xn])
                        kb.op(act, lambda: A.copy(xnb[:], xn[:]), [b_xn], [b_xnb])
                        for half in range(2):
                            bi = half
                            for q in range(4):
                                dt = half * 4 + q
                                kb.op(pe, lambda: T.matmul(bank[bi][:, q * 128:(q + 1) * 128], xnb[:, dt * 128:(dt + 1) * 128], idb[:],
                                                           start=True, stop=True), [b_xnb, b_idb], [b_bank[bi]])
                            evac(xT[:, half * 4:half * 4 + 4, :], bank[bi].rearrange("p (a b) -> p a b", a=4), [b_bank[bi]], [b_xT], force_act=True)
                        for g4 in range(4):
                            bi = 2 + g4
                            for q in range(4):
                                hp = g4 * 4 + q
                                for dt in range(8):
                                    kb.op(pe, lambda: T.matmul(bank[bi][:, q * 128:(q + 1) * 128], WQ[:, dt, hp * 128:(hp + 1) * 128],
                                                               xT[:, dt, :], start=(dt == 0), stop=(dt == 7)), [b_WQ, b_xT], [b_bank[bi]])
                            evac(qTt[:, g4 * 4:g4 * 4 + 4, :], bank[bi].rearrange("p (a b) -> p a b", a=4), [b_bank[bi]], [b_qTt], force_act=True)
                        for g4 in range(4):
                            bi = g4
                            for q in range(4):
                                hp = g4 * 4 + q
                                kb.op(pe, lambda: T.matmul(bank[bi][:, q * 128:(q + 1) * 128], qTt[:, hp, :], kTt[:, hp, :],
                                                           start=True, stop=True), [b_qTt, b_kTt], [b_bank[bi]])
                            evac(sc[:, g4 * 4:g4 * 4 + 4, :], bank[bi].rearrange("p (a b) -> p a b", a=4), [b_bank[bi]], [b_sc], force_act=True)
                def f_b(tt):
                        x1t, b_x1t = X1[tt % 2]
                        xn, b_xn = XN[tt % 2]
                        idxu, b_idxu = IDX[tt % 2]
                        gw, b_gw = GW[tt % 2]
                        gwf = gw.rearrange("p h k -> p (h k)")
                        r0 = tt * 128
                        sv4 = svl.rearrange("p (h two) k -> p h two k", two=2)
                        si4 = sif.rearrange("p (h two) k -> p h two k", two=2)
                        c4 = cand.rearrange("p h (i j) -> p h i j", i=16)
                        B4 = [128, 8, 16, 16]
                        for hp in range(16):
                            kb.op(dve, lambda: V.max(svl[:, hp, 0:8], sc[:, hp, :]), [b_sc], [b_svl])
                            kb.op(dve, lambda: V.match_replace(sc2[:, hp, :], svl[:, hp, 0:8], sc[:, hp, :], -1e30), [b_svl, b_sc], [b_sc2])
                            kb.op(dve, lambda: V.max(svl[:, hp, 8:16], sc2[:, hp, :]), [b_sc2], [b_svl])
                            kb.op(dve, lambda: V.max_index(siu[:, hp, 0:8], svl[:, hp, 0:8], sc[:, hp, :]), [b_svl, b_sc], [b_siu])
                            kb.op(dve, lambda: V.max_index(siu[:, hp, 8:16], svl[:, hp, 8:16], sc2[:, hp, :]), [b_svl, b_sc2], [b_siu])
                        kb.op(act, lambda: A.copy(sif[:], siu[:]), [b_siu], [b_sif])
                def f_c(tt):
                        x1t, b_x1t = X1[tt % 2]
                        xn, b_xn = XN[tt % 2]
                        idxu, b_idxu = IDX[tt % 2]
                        gw, b_gw = GW[tt % 2]
                        gwf = gw.rearrange("p h k -> p (h k)")
                        r0 = tt * 128
                        sv4 = svl.rearrange("p (h two) k -> p h two k", two=2)
                        si4 = sif.rearrange("p (h two) k -> p h two k", two=2)
                        c4 = cand.rearrange("p h (i j) -> p h i j", i=16)
                        B4 = [128, 8, 16, 16]
                        sv4 = svl.rearrange("p (h two) k -> p h two k", two=2)
                        si4 = sif.rearrange("p (h two) k -> p h two k", two=2)
                        c4 = cand.rearrange("p h (i j) -> p h i j", i=16)
                        kb.op(dve, lambda: V.tensor_tensor(c4[:], sv4[:, :, 0, :].unsqueeze(3).to_broadcast(B4),
                                                           sv4[:, :, 1, :].unsqueeze(2).to_broadcast(B4), ALU.add),
                              [b_svl], [b_cand])
                        for h in range(8):
                            kb.op(dve, lambda: V.max(ts[:, h, 0:8], cand[:, h, :]), [b_cand], [b_ts])
                            kb.op(dve, lambda: V.match_replace(cand2[:, h, :], ts[:, h, 0:8], cand[:, h, :], -1e30), [b_ts, b_cand], [b_cand2])
                            kb.op(dve, lambda: V.max(ts[:, h, 8:16], cand2[:, h, :]), [b_cand2], [b_ts])
                            kb.op(dve, lambda: V.max_index(pu32[:, h, 0:8], ts[:, h, 0:8], cand[:, h, :]), [b_ts, b_cand], [b_pu32])
                            kb.op(dve, lambda: V.max_index(pu32[:, h, 8:16], ts[:, h, 8:16], cand2[:, h, :]), [b_ts, b_cand2], [b_pu32])
                        kb.op(dve, lambda: V.tensor_scalar(nmx[:], ts[:, :, 0], -1.0, None, ALU.mult), [b_ts], [b_nmx])
                        for h in range(8):
                            kb.op(act, lambda: A.activation(gw[:, h, :], ts[:, h, :], AF.Exp, bias=nmx[:, h:h + 1], accum_out=zs[:, h:h + 1]),
                                  [b_ts, b_nmx], [b_gw, b_zs])
                def f_d(tt):
                        x1t, b_x1t = X1[tt % 2]
                        xn, b_xn = XN[tt % 2]
                        idxu, b_idxu = IDX[tt % 2]
                        gw, b_gw = GW[tt % 2]
                        gwf = gw.rearrange("p h k -> p (h k)")
                        r0 = tt * 128
                        sv4 = svl.rearrange("p (h two) k -> p h two k", two=2)
                        si4 = sif.rearrange("p (h two) k -> p h two k", two=2)
                        c4 = cand.rearrange("p h (i j) -> p h i j", i=16)
                        B4 = [128, 8, 16, 16]
                        kb.op(dve, lambda: V.reciprocal(zs[:], zs[:]), [b_zs], [b_zs])
                        kb.op(dve, lambda: V.tensor_tensor(gw[:], gw[:], zs[:].unsqueeze(2).to_broadcast([128, 8, 16]), ALU.mult),
                              [b_gw, b_zs], [b_gw])
                        kb.op(dve, lambda: V.tensor_copy(posf[:], pu32[:]), [b_pu32], [b_posf])
                        B4 = [128, 8, 16, 16]
                        kb.op(dve, lambda: V.tensor_tensor(eq[:], posf[:].unsqueeze(3).to_broadcast(B4),
                                                           thr16[:].unsqueeze(1).unsqueeze(1).to_broadcast(B4), ALU.is_ge),
                              [b_posf, b_io16], [b_eq])
                        kb.op(dve, lambda: V.tensor_reduce(pi_[:], eq[:], AX.X, ALU.add), [b_eq], [b_pi])
                        kb.op(dve, lambda: V.scalar_tensor_tensor(pj[:], pi_[:], -16.0, posf[:], ALU.mult, ALU.add), [b_pi, b_posf], [b_pj])
                        for (src, b_src, two, dst, b_dst) in ((pi_, b_pi, 0, I1, b_I1), (pj, b_pj, 1, I2, b_I2)):
                            kb.op(dve, lambda: V.tensor_tensor(eq[:], src[:].unsqueeze(3).to_broadcast(B4),
                                                               io16[:].unsqueeze(1).unsqueeze(1).to_broadcast(B4), ALU.is_equal),
                                  [b_src, b_io16], [b_eq])
                            kb.op(dve, lambda: V.tensor_tensor(eq[:], eq[:], si4[:, :, two, :].unsqueeze(2).to_broadcast(B4), ALU.mult),
                                  [b_eq, b_sif], [b_eq])
                            kb.op(dve, lambda: V.tensor_reduce(dst[:], eq[:], AX.X, ALU.add), [b_eq], [b_dst])
                        kb.op(dve, lambda: V.scalar_tensor_tensor(I1[:], I1[:], 128.0, I2[:], ALU.mult, ALU.add), [b_I1, b_I2], [b_I1])
                        kb.op(dve, lambda: V.tensor_copy(idxu[:], I1[:].rearrange("p h k -> p (h k)")), [b_I1], [b_idxu])
                def f_back(tt):
                        x1t, b_x1t = X1[tt % 2]
                        xn, b_xn = XN[tt % 2]
                        idxu, b_idxu = IDX[tt % 2]
                        gw, b_gw = GW[tt % 2]
                        gwf = gw.rearrange("p h k -> p (h k)")
                        r0 = tt * 128
                        sv4 = svl.rearrange("p (h two) k -> p h two k", two=2)
                        si4 = sif.rearrange("p (h two) k -> p h two k", two=2)
                        c4 = cand.rearrange("p h (i j) -> p h i j", i=16)
                        B4 = [128, 8, 16, 16]
                        GRP = 4

                        def consume(g0, tiles):
                            gs = slice(g0, g0 + GRP)
                            kb.op(pool, lambda: P.tensor_tensor(wgt[:, gs], wgt[:, gs], gwf[:, gs], ALU.mult), [BW[g0 // GRP], b_gw], [BW[g0 // GRP]])
                            for i, hk in enumerate(range(g0, g0 + GRP)):
                                ut, b_ut = tiles[i]
                                dg, b_dg = dgr.next()
                                kb.op(act, lambda: A.activation(dg[:], idf[:], AF.Copy, scale=wgt[:, hk:hk + 1]), [b_idf, BW[g0 // GRP]], [b_dg])
                                kb.op_multi(pe, [
                                    lambda: T.matmul(bank[6], dg[:], ut[:, D:D + 512], start=(hk == 0), stop=(hk == 127)),
                                    lambda: T.matmul(bank[7], dg[:], ut[:, D + 512:2 * D], start=(hk == 0), stop=(hk == 127)),
                                ], [b_dg, b_ut], [b_bank[6], b_bank[7]])

                        prev = None
                        for g0 in range(0, 128, GRP):
                            if nxt_stage.get(g0) is not None and tt + 1 < NTILES:
                                nxt_stage[g0](tt + 1)
                            tiles = []
                            for hk in range(g0, g0 + GRP):
                                ut, b_ut = ur.next()
                                tiles.append((ut, b_ut))
                                kb.dma(pool, ut[:], puv, b_puv, b_ut, indirect=(idxu[:, hk:hk + 1], b_idxu))
                                kb.op(dve, lambda: V.scalar_tensor_tensor(junk[:], ut[:, 0:D], 1.0, xn[:], ALU.mult, ALU.mult,
                                                                          accum_out=actt[:, hk:hk + 1]), [b_ut, b_xn], [b_junk, BA[g0 // GRP]])
                            gs = slice(g0, g0 + GRP)
                            kb.op(act, lambda: A.activation(wgt[:, gs], actt[:, gs], AF.Gelu_apprx_tanh), [BA[g0 // GRP]], [BW[g0 // GRP]])
                            if prev is not None:
                                consume(*prev)
                            prev = (g0, tiles)
                        consume(*prev)
                        for half in range(2):
                            kb.op(dve, lambda: V.tensor_tensor(acc[:, half * 512:(half + 1) * 512], bank[6 + half],
                                                               x1t[:, half * 512:(half + 1) * 512], ALU.add),
                                  [b_bank[6 + half], b_x1t], [b_acc])
                        kb.op(act, lambda: A.activation(junk[:], acc[:], AF.Square, accum_out=ss[:, 1:2]), [b_acc], [b_junk, b_ss])
                        kb.op(act, lambda: A.activation(ss[:, 1:2], ss[:, 1:2], AF.Sqrt, bias=epsb[:], scale=1.0 / D), [b_ss, b_epsb], [b_ss])
                        kb.op(dve, lambda: V.reciprocal(ss[:, 1:2], ss[:, 1:2]), [b_ss], [b_ss])
                        kb.op(dve, lambda: V.scalar_tensor_tensor(yt[:], acc[:], ss[:, 1:2], gf[:], ALU.mult, ALU.mult),
                              [b_acc, b_ss, b_gf], [b_yt])
                        kb.dma(sp, y[r0:r0 + 128, :], yt[:], b_yt, b_y)
                NTILES = SO // 128
                nxt_stage = {0: f_a0, 8: f_a, 40: f_b, 72: f_c, 104: f_d}
                for st in (f_a0, f_a, f_b, f_c, f_d):
                    st(0)
                for tt in range(NTILES):
                    f_back(tt)
                kb.barrier()
        kb.barrier()
        build.stats = (kb.ninst, kb.nwait, kb.nsem)
    return nc


def _rel_bucket_np(rel):
    nb = 16
    ret = np.where(rel > 0, nb, 0).astype(np.int32)
    n = np.abs(rel)
    max_exact = 8
    nf = np.maximum(n, 1).astype(np.float32)
    large = max_exact + (np.log(nf / max_exact) / np.float32(math.log(128 / max_exact)) * (nb - max_exact)).astype(np.int32)
    large = np.minimum(large, nb - 1)
    return ret + np.where(n < max_exact, n, large)


def host_consts():
    r = np.arange(512)
    rel = 256 - r
    b = _rel_bucket_np(rel)
    ohb = np.zeros((32, 512), np.float32)
    ohb[b, r] = 1.0
    return {
        "ident": np.eye(128, dtype=np.float32),
        "antiid": np.ascontiguousarray(np.eye(128, dtype=np.float32)[::-1]),
        "ohb": ohb,
    }


def squeeze_weights(inputs):
    out = {}
    for n, s in WNAMES:
        a = np.asarray(inputs[n], dtype=np.float32)
        out[n] = np.ascontiguousarray(a.reshape(s))
    return out


def make_in_maps(x_prompt, x_sample, weights, SO):
    consts = host_consts()
    maps = []

    def mk(xf, xo, fl):
        m = {"xf": np.ascontiguousarray(xf, dtype=np.float32), "xo": np.ascontiguousarray(xo, dtype=np.float32),
             "flags": np.tile(np.asarray(fl, np.float32)[None, :], (128, 1))}
        m.update(consts)
        m.update(weights)
        return m

    for b in range(x_prompt.shape[0]):
        xs = x_prompt[b]
        xf = np.concatenate([xs, np.zeros_like(xs)], axis=0)
        maps.append(mk(xf, xs, [1, 0, 1, 0]))
    for b in range(x_sample.shape[0]):
        xs = x_sample[b]
        maps.append(mk(xs, xs[:SO], [1, 0, 0, 0]))
        maps.append(mk(xs, xs[SO:], [0, 1, 0, 0]))
    return maps


_NC_CACHE = {}


def kernel(**inputs):
    x_prompt = np.asarray(inputs["x_prompt"], np.float32)
    x_sample = np.asarray(inputs["x_sample"], np.float32)
    SO = x_prompt.shape[1]
    assert x_sample.shape[1] == 2 * SO
    weights = squeeze_weights(inputs)
    maps = make_in_maps(x_prompt, x_sample, weights, SO)
    n = len(maps)
    if SO not in _NC_CACHE:
        _NC_CACHE[SO] = build(SO)
    nc = _NC_CACHE[SO]
    res = run_bass_kernel_spmd(nc, maps, core_ids=list(range(n)))
    outs = [np.asarray(r["y"], np.float32) for r in res.results]
    nb = x_prompt.shape[0]
    y_prompt = np.stack(outs[:nb], axis=0)
    y_sample = np.stack([np.concatenate([outs[nb + 2 * b], outs[nb + 2 * b + 1]], axis=0) for b in range(x_sample.shape[0])], axis=0)
    return (y_prompt, y_sample)
```

```python
from contextlib import ExitStack
import math
import numpy as np
import concourse.bass as bass
import concourse.mybir as mybir
from concourse.bass_utils import run_bass_kernel_spmd

F32 = mybir.dt.float32
BF16 = mybir.dt.bfloat16
I32 = mybir.dt.int32
U32 = mybir.dt.uint32
AF = mybir.ActivationFunctionType
ALU = mybir.AluOpType
AX = mybir.AxisListType

D = 1024
NH = 8
EPS = 1e-6
LAM_INIT = 0.2
NEG = -30000.0


class Buf:
    __slots__ = ("name", "last_w", "readers", "dram", "sem", "cnt")

    def __init__(self, name, dram=False):
        self.name = name
        self.last_w = None
        self.readers = []
        self.dram = dram
        self.sem = None
        self.cnt = 0


class Eng:
    def __init__(self, name, handle, sem, self_sync=True):
        self.name = name
        self.h = handle
        self.sem = sem
        self.count = 0
        self.seen = {}
        self.self_sync = self_sync


class KB:
    def __init__(self, nc, es):
        self.nc = nc
        self.es = es
        self.nsem = 0
        self.pe = Eng("pe", nc.tensor, self.newsem("s_pe"), self_sync=False)
        self.dve = Eng("dve", nc.vector, self.newsem("s_dve"))
        self.act = Eng("act", nc.scalar, self.newsem("s_act"))
        self.pool = Eng("pool", nc.gpsimd, self.newsem("s_pool"))
        self.sp = Eng("sp", nc.sync, self.newsem("s_sp"))
        self.engs = [self.pe, self.dve, self.act, self.pool, self.sp]
        self.dbufs = []
        self.nwait = 0
        self.ninst = 0

    def newsem(self, name):
        self.nsem += 1
        return self.es.enter_context(self.nc.semaphore(f"{name}_{self.nsem}"))

    def sb(self, name, shape, dtype, es=None):
        return (es or self.es).enter_context(self.nc.sbuf_tensor(name, list(shape), dtype))

    def ps(self, name, shape, dtype):
        return self.es.enter_context(self.nc.psum_tensor(name, list(shape), dtype))

    def _wait(self, eng, tok):
        sem, val, src = tok
        if src is eng and not eng.self_sync:
            return
        key = id(sem)
        if eng.seen.get(key, 0) >= val:
            return
        eng.h.wait_ge(sem, val)
        eng.seen[key] = val
        self.nwait += 1

    def _deps(self, eng, reads, writes):
        for b in reads:
            if b.last_w is not None:
                self._wait(eng, b.last_w)
        for b in writes:
            if b.dram:
                continue
            if b.last_w is not None:
                self._wait(eng, b.last_w)
            for t in b.readers:
                self._wait(eng, t)

    def op(self, eng, fn, reads=(), writes=()):
        self._deps(eng, reads, writes)
        ins = fn()
        ins.then_inc(eng.sem, 1)
        eng.count += 1
        self.ninst += 1
        tok = (eng.sem, eng.count, eng)
        for b in writes:
            b.last_w = tok
            b.readers = []
        for b in reads:
            if not b.dram:
                b.readers.append(tok)
                if len(b.readers) > 48:
                    b.readers = b.readers[-48:]
        return tok

    def op_multi(self, eng, fns, reads=(), writes=()):
        self._deps(eng, reads, writes)
        ins = None
        for fn in fns:
            ins = fn()
            self.ninst += 1
        ins.then_inc(eng.sem, 1)
        eng.count += 1
        tok = (eng.sem, eng.count, eng)
        for b in writes:
            b.last_w = tok
            b.readers = []
        for b in reads:
            if not b.dram:
                b.readers.append(tok)
                if len(b.readers) > 48:
                    b.readers = b.readers[-48:]
        return tok

    def dma(self, eng, out, in_, src, dst, indirect=None, **kw):
        reads = [src] if src is not None else []
        writes = [dst] if dst is not None else []
        if indirect is not None:
            reads = reads + [indirect[1]]
        cb = dst if dst is not None else src
        if dst is not None and not dst.dram and dst.last_w is not None and dst.last_w[2] is None \
                and dst.last_w[0] is dst.sem and not dst.readers:
            saved = dst.last_w
            dst.last_w = None
            self._deps(eng, reads, writes)
            dst.last_w = saved
        else:
            self._deps(eng, reads, writes)
        if cb.sem is None:
            cb.sem = self.newsem("d_" + cb.name)
            self.dbufs.append(cb)
        if indirect is not None:
            ins = eng.h.indirect_dma_start(out=out, out_offset=None, in_=in_,
                                           in_offset=bass.IndirectOffsetOnAxis(ap=indirect[0], axis=0))
        else:
            ins = eng.h.dma_start(out=out, in_=in_, **kw)
        cb.cnt += 1
        ins.then_inc(cb.sem, 16)
        self.ninst += 1
        tok = (cb.sem, 16 * cb.cnt, None)
        if dst is not None:
            dst.last_w = tok
            if not dst.dram:
                dst.readers = []
        for b in reads:
            if not b.dram:
                b.readers.append(tok)
        return tok

    def barrier(self):
        toks = [(e.sem, e.count, None) for e in self.engs if e.count > 0]
        toks += [(b.sem, 16 * b.cnt, None) for b in self.dbufs if b.cnt > 0]
        for e in self.engs:
            for t in toks:
                if t[0] is e.sem:
                    continue
                self._wait(e, t)


class Ring:
    def __init__(self, kb, name, n, shape, dtype, es=None):
        self.items = [(kb.sb(f"{name}{i}", shape, dtype, es), Buf(f"{name}{i}")) for i in range(n)]
        self.i = 0

    def next(self):
        it = self.items[self.i % len(self.items)]
        self.i += 1
        return it


WNAMES = [
    ("norm1_g", [D]), ("w_in", [D, 7168]), ("b_gate", [2048]), ("conv_w", [4, D]), ("conv_b", [D]),
    ("lru_wa_f", [4, 256, 256]), ("lru_ba_f", [D]), ("lru_wx_f", [4, 256, 256]), ("lru_bx_f", [D]),
    ("lru_lam_f", [D]),
    ("lru_wa_b", [4, 256, 256]), ("lru_ba_b", [D]), ("lru_wx_b", [4, 256, 256]), ("lru_bx_b", [D]),
    ("lru_lam_b", [D]),
    ("lam_q1", [64]), ("lam_k1", [64]), ("lam_q2", [64]), ("lam_k2", [64]), ("subln_g", [128]),
    ("rel_bias", [32, 8]), ("w_rnn_out", [D, D]), ("w_attn_out", [D, D]), ("w_out", [D, D]),
    ("norm2_g", [D]), ("peer_wq", [D, 2048]), ("peer_keys", [16, 128, 128]),
    ("peer_u", [16384, D]), ("peer_v", [16384, D]), ("final_g", [D]),
]


def build(SO, phases="ABCDEF", debug=False):
    SF = 2 * SO
    NQC = SO // 512
    NKB = SF // 128
    OFFB = SO // 128
    assert OFFB >= 8
    TC = min(2048, SO)
    NTC = SF // TC
    nc = bass.Bass("TRN2", target_bir_lowering=False)

    def din(name, shape, dt=F32):
        return nc.dram_tensor(name, list(shape), dt, kind="ExternalInput").ap()

    def dscr(name, shape, dt):
        return nc.dram_tensor(name, list(shape), dt, kind="ExternalOutput" if debug else "Internal").ap()

    xf = din("xf", [SF, D])
    xo = din("xo", [SO, D])
    flags_d = din("flags", [128, 4])
    ident_d = din("ident", [128, 128])
    anti_d = din("antiid", [128, 128])
    ohb_d = din("ohb", [32, 512])
    W = {n: din(n, s) for n, s in WNAMES}
    y = nc.dram_tensor("y", [SO, D], F32, kind="ExternalOutput").ap()

    xrT = dscr("xrT", [D, SF], F32); b_xrT = Buf("xrT", True)
    kT = dscr("kT", [D, SF], BF16); b_kT = Buf("kT", True)
    vv = dscr("vv", [SF, D], BF16); b_vv = Buf("vv", True)
    qT = dscr("qT", [D, SO], BF16); b_qT = Buf("qT", True)
    ggT = dscr("ggT", [D, SO], F32); b_ggT = Buf("ggT", True)
    grT = dscr("grT", [D, SO], F32); b_grT = Buf("grT", True)
    gaT = dscr("gaT", [D, SO], F32); b_gaT = Buf("gaT", True)
    hf = dscr("hf", [D, SF], F32); b_hf = Buf("hf", True)
    hb = dscr("hb", [D, SF], F32); b_hb = Buf("hb", True)
    oT = dscr("oT", [D, SO], BF16); b_oT = Buf("oT", True)
    x1 = dscr("x1", [SO, D], F32); b_x1 = Buf("x1", True)
    tvd = dscr("tvd", [8, 512], F32); b_tvd = Buf("tvd", True)
    puv = dscr("puv", [16384, 2 * D], BF16); b_puv = Buf("puv", True)
    b_y = Buf("y", True)

    with ExitStack() as es:
        kb = KB(nc, es)
        pe, dve, act, pool, sp = kb.pe, kb.dve, kb.act, kb.pool, kb.sp
        V, A, P, T = nc.vector, nc.scalar, nc.gpsimd, nc.tensor

        psA = kb.ps("psA", [128, 2048], F32)
        psB = kb.ps("psB", [128, 2048], F32)
        bank = [(psA if i < 4 else psB)[:, (i % 4) * 512:(i % 4) * 512 + 512] for i in range(8)]
        b_bank = [Buf(f"bank{i}") for i in range(8)]

        def ctile(name, shape, dt=F32):
            return kb.sb(name, shape, dt), Buf(name)

        idf, b_idf = ctile("idf", [128, 128])
        idb, b_idb = ctile("idb", [128, 128], BF16)
        anf, b_anf = ctile("anf", [128, 128])
        anb, b_anb = ctile("anb", [128, 128], BF16)
        onesb, b_onesb = ctile("onesb", [128, 128], BF16)
        onesf, b_onesf = ctile("onesf", [128, 128])
        flg, b_flg = ctile("flg", [128, 8])
        epsb, b_epsb = ctile("epsb", [128, 1])
        kb.dma(sp, idf[:], ident_d, None, b_idf)
        kb.dma(sp, anf[:], anti_d, None, b_anf)
        kb.dma(sp, flg[:, 0:4], flags_d, None, b_flg)
        kb.op(dve, lambda: V.tensor_copy(idb[:], idf[:]), [b_idf], [b_idb])
        kb.op(dve, lambda: V.tensor_copy(anb[:], anf[:]), [b_anf], [b_anb])
        kb.op(dve, lambda: V.memset(onesb[:], 1.0), [], [b_onesb])
        kb.op(dve, lambda: V.memset(onesf[:], 1.0), [], [b_onesf])
        kb.op(dve, lambda: V.memset(epsb[:], EPS), [], [b_epsb])
        kb.op(dve, lambda: V.tensor_scalar(flg[:, 3:4], flg[:, 2:3], -1.0, 1.0, ALU.mult, ALU.add), [b_flg], [b_flg])
        kb.op(dve, lambda: V.tensor_scalar(flg[:, 4:5], flg[:, 2:3], NEG, None, ALU.mult), [b_flg], [b_flg])
        fA, fB, fP, vP, msk = (flg[:, i:i + 1] for i in range(5))

        def bcast_load(dst_ap, src_1d, n, b_dst):
            kb.dma(sp, dst_ap, src_1d.rearrange("(o n) -> o n", o=1).to_broadcast([128, n]), None, b_dst)

        def chan_load(dst_ap, src_1d, b_dst):
            kb.dma(sp, dst_ap, src_1d.rearrange("(t p) -> p t", p=128), None, b_dst,
                   allow_slow_non_contiguous=True)

        rr = {"evac": 0}

        def evac(out_ap, in_ap, reads, writes, force_act=False):
            rr["evac"] += 1
            if rr["evac"] % 2 and not force_act:
                kb.op(dve, lambda: V.tensor_copy(out_ap, in_ap), reads, writes)
            else:
                kb.op(act, lambda: A.copy(out_ap, in_ap), reads, writes)

        def norm_pieces(xsrc, t0, gbc, b_gbc, xring, xnring, uT, b_uT, ssr, junk, b_junk, bsel):
            st = {}

            def pA(tt):
                xt, b_xt = xring.next()
                kb.dma(sp, xt[:], xsrc[t0 + tt * 128:t0 + (tt + 1) * 128, :], None, b_xt)
                ss, b_ss = ssr.next()
                kb.op(act, lambda: A.activation(junk[:], xt[:], AF.Square, accum_out=ss[:]), [b_xt], [b_junk, b_ss])
                kb.op(act, lambda: A.activation(ss[:], ss[:], AF.Sqrt, bias=epsb[:], scale=1.0 / D), [b_ss, b_epsb], [b_ss])
                kb.op(dve, lambda: V.reciprocal(ss[:], ss[:]), [b_ss], [b_ss])
                xn, b_xn = xnring.next()
                kb.op(dve, lambda: V.scalar_tensor_tensor(xn[:], xt[:], ss[:, 0:1], gbc[:], ALU.mult, ALU.mult),
                      [b_xt, b_ss, b_gbc], [b_xn])
                st[tt] = (xn, b_xn)

            def pB(tt):
                xn, b_xn = st.pop(tt)
                for half in range(2):
                    bi = bsel[(tt * 2 + half) % len(bsel)]
                    kb.op_multi(pe, [lambda q=q: T.matmul(bank[bi][:, q * 128:(q + 1) * 128],
                                                          xn[:, (half * 4 + q) * 128:(half * 4 + q + 1) * 128],
                                                          idb[:], start=True, stop=True) for q in range(4)],
                                [b_xn, b_idb], [b_bank[bi]])
                    evac(uT[:, half * 4:half * 4 + 4, tt * 128:(tt + 1) * 128],
                         bank[bi].rearrange("p (a b) -> p a b", a=4), [b_bank[bi]], [b_uT])
            return pA, pB

        def pipelined_chunks(nchunks, xsrc, ngroups, body, gbc, b_gbc, xring, xnring, uTr, ssr, junk, b_junk):
            def prep(ch):
                uT, b_uT = uTr.next()
                return (uT, b_uT) + norm_pieces(xsrc, ch * 512, gbc, b_gbc, xring, xnring, uT, b_uT, ssr, junk, b_junk, [0, 1])
            cur = prep(0)
            for tt in range(4):
                cur[2](tt)
                cur[3](tt)
            step = max(ngroups // 4, 2)
            for ch in range(nchunks):
                nxt = prep(ch + 1) if ch + 1 < nchunks else None
                hooks = {}
                if nxt is not None:
                    for tt in range(4):
                        hooks[1 + step * tt] = (nxt[2], tt)
                        hooks[min(step - 2 + step * tt + 1, ngroups)] = (nxt[3], tt)
                cnt = {"g": 0}

                def tick():
                    cnt["g"] += 1
                    h_ = hooks.pop(cnt["g"], None)
                    if h_ is not None:
                        h_[0](h_[1])
                body(ch, cur[0], cur[1], tick)
                for g_ in sorted(hooks):
                    hooks[g_][0](hooks[g_][1])
                cur = nxt

        def convert_tables(pes):
            cvr = Ring(kb, "cv", 2, [128, 8, D], BF16, pes)
            for ti, nm in enumerate(("peer_u", "peer_v")):
                src = W[nm].rearrange("(p r) d -> p r d", p=128)
                dst = puv.rearrange("(p r) d -> p r d", p=128)
                for r0 in range(0, 128, 8):
                    cv, b_cv = cvr.next()
                    kb.dma(pool, cv[:], src[:, r0:r0 + 8, :], None, b_cv)
                    kb.dma(sp, dst[:, r0:r0 + 8, ti * D:(ti + 1) * D], cv[:], b_cv, b_puv)

        if "B" in phases:
            with ExitStack() as pes:
                gbc, b_gbc = kb.sb("gbc", [128, D], F32, pes), Buf("gbc")
                bcast_load(gbc[:], W["norm1_g"], D, b_gbc)
                xring = Ring(kb, "xt", 3, [128, D], F32, pes)
                xnring = Ring(kb, "xn", 3, [128, D], BF16, pes)
                ssr = Ring(kb, "ss", 3, [128, 1], F32, pes)
                junk, b_junk = kb.sb("junk", [128, D], BF16, pes), Buf("junk")
                uTr = Ring(kb, "uT", 2, [128, 8, 512], BF16, pes)
                bg, b_bg = kb.sb("bg", [128, 16], F32, pes), Buf("bg")
                kb.dma(sp, bg[:], W["b_gate"].rearrange("(t p) -> p t", p=128), None, b_bg, allow_slow_non_contiguous=True)
                with ExitStack() as pa:
                    WA, b_WA = kb.sb("WA", [128, 8, 3072], BF16, pa), Buf("WA")
                    for dt in range(8):
                        for g, c0 in enumerate((0, 3072, 4096)):
                            kb.dma(pool, WA[:, dt, g * 1024:(g + 1) * 1024], W["w_in"][dt * 128:(dt + 1) * 128, c0:c0 + 1024],
                                   None, b_WA)
                    sxr = Ring(kb, "sx", 2, [128, 8, 512], F32, pa)
                    skr = Ring(kb, "sk", 2, [128, 8, 512], BF16, pa)
                    svr = Ring(kb, "sv", 2, [128, 4, 1024], BF16, pa)
                    def bodyA(ch, uT, b_uT, tick):
                        t0 = ch * 512
                        sx, b_sx = sxr.next()
                        sk, b_sk = skr.next()
                        sv, b_sv = svr.next()
                        for j in range(16):
                            bi = 2 + j % 6
                            kb.op_multi(pe, [lambda dt=dt: T.matmul(bank[bi], WA[:, dt, j * 128:(j + 1) * 128], uT[:, dt, :],
                                                                    start=(dt == 0), stop=(dt == 7)) for dt in range(8)],
                                        [b_WA, b_uT], [b_bank[bi]])
                            if j < 8:
                                evac(sx[:, j, :], bank[bi], [b_bank[bi]], [b_sx])
                            else:
                                evac(sk[:, j - 8, :], bank[bi], [b_bank[bi]], [b_sk])
                            tick()
                        for tt in range(4):
                            for half in range(2):
                                bi = 2 + (tt * 2 + half) % 6
                                kb.op_multi(pe, [lambda dt=dt: T.matmul(bank[bi], uT[:, dt, tt * 128:(tt + 1) * 128],
                                                                        WA[:, dt, 2048 + half * 512:2048 + (half + 1) * 512],
                                                                        start=(dt == 0), stop=(dt == 7)) for dt in range(8)],
                                            [b_WA, b_uT], [b_bank[bi]])
                                evac(sv[:, tt, half * 512:(half + 1) * 512], bank[bi], [b_bank[bi]], [b_sv])
                                tick()
                        kb.dma(sp, xrT.rearrange("(c p) t -> p c t", p=128)[:, :, t0:t0 + 512], sx[:], b_sx, b_xrT)
                        kb.dma(sp, kT.rearrange("(c p) t -> p c t", p=128)[:, :, t0:t0 + 512], sk[:], b_sk, b_kT)
                        kb.dma(sp, vv[t0:t0 + 512, :].rearrange("(a p) e -> p a e", p=128), sv[:], b_sv, b_vv)

                    pipelined_chunks(SF // 512, xf, 24, bodyA, gbc, b_gbc, xring, xnring, uTr, ssr, junk, b_junk)
                    kb.barrier()
                with ExitStack() as pb:
                    WB, b_WB = kb.sb("WB", [128, 8, 4096], BF16, pb), Buf("WB")
                    for dt in range(8):
                        for g, c0 in enumerate((1024, 2048, 5120, 6144)):
                            kb.dma(pool, WB[:, dt, g * 1024:(g + 1) * 1024], W["w_in"][dt * 128:(dt + 1) * 128, c0:c0 + 1024],
                                   None, b_WB)
                    sgr = Ring(kb, "sg", 1, [128, 8, 512], F32, pb)
                    sqr = Ring(kb, "sq", 1, [128, 8, 512], BF16, pb)
                    srr = Ring(kb, "sr", 1, [128, 8, 512], F32, pb)
                    sar = Ring(kb, "sa", 1, [128, 8, 512], F32, pb)
                    def bodyB(ch, uT, b_uT, tick):
                        t0 = ch * 512
                        sg, b_sg = sgr.next()
                        sq, b_sq = sqr.next()
                        sr, b_sr = srr.next()
                        sa, b_sa = sar.next()
                        for j in range(32):
                            bi = 2 + j % 6
                            kb.op_multi(pe, [lambda dt=dt: T.matmul(bank[bi], WB[:, dt, j * 128:(j + 1) * 128], uT[:, dt, :],
                                                                    start=(dt == 0), stop=(dt == 7)) for dt in range(8)],
                                        [b_WB, b_uT], [b_bank[bi]])
                            jj = j % 8
                            if j < 8:
                                kb.op(act, lambda: A.activation(sg[:, jj, :], bank[bi], AF.Gelu_apprx_tanh), [b_bank[bi]], [b_sg])
                            elif j < 16:
                                kb.op(dve, lambda: V.tensor_copy(sq[:, jj, :], bank[bi]), [b_bank[bi]], [b_sq])
                            elif j < 24:
                                kb.op(act, lambda: A.activation(sr[:, jj, :], bank[bi], AF.Sigmoid, bias=bg[:, jj:jj + 1]),
                                      [b_bank[bi], b_bg], [b_sr])
                            else:
                                kb.op(act, lambda: A.activation(sa[:, jj, :], bank[bi], AF.Sigmoid, bias=bg[:, 8 + jj:9 + jj]),
                                      [b_bank[bi], b_bg], [b_sa])
                            tick()
                        for dst, b_dst, s_, b_s in ((ggT, b_ggT, sg, b_sg), (qT, b_qT, sq, b_sq), (grT, b_grT, sr, b_sr),
                                                    (gaT, b_gaT, sa, b_sa)):
                            kb.dma(sp, dst.rearrange("(c p) t -> p c t", p=128)[:, :, t0:t0 + 512], s_[:], b_s, b_dst)

                    pipelined_chunks(SO // 512, xo, 32, bodyB, gbc, b_gbc, xring, xnring, uTr, ssr, junk, b_junk)
                    kb.barrier()

        if "C" in phases:
            with ExitStack() as pes:
                cw, b_cw = kb.sb("cw", [128, 4, 8], F32, pes), Buf("cw")
                for j in range(4):
                    chan_load(cw[:, j, :], W["conv_w"][j, :], b_cw)
                cpar, b_cpar = kb.sb("cpar", [128, 7, 8], F32, pes), Buf("cpar")
                for i, nm in enumerate(("conv_b", "lru_ba_f", "lru_bx_f", "lru_ba_b", "lru_bx_b", "lru_lam_f", "lru_lam_b")):
                    chan_load(cpar[:, i, :], W[nm], b_cpar)
                kb.op(act, lambda: A.activation(cpar[:, 5:7, :], cpar[:, 5:7, :], AF.Exp, scale=-1.0), [b_cpar], [b_cpar])
                kb.op(act, lambda: A.activation(cpar[:, 5:7, :], cpar[:, 5:7, :], AF.Ln, bias=1.0), [b_cpar], [b_cpar])
                kb.op(dve, lambda: V.tensor_scalar(cpar[:, 5:7, :], cpar[:, 5:7, :], -8.0, None, ALU.mult), [b_cpar], [b_cpar])
                GW, b_GW = kb.sb("GW", [128, 4, 8, 256], BF16, pes), Buf("GW")
                for gi, nm in enumerate(("lru_wa_f", "lru_wx_f", "lru_wa_b", "lru_wx_b")):
                    for n in range(4):
                        kb.dma(pool, GW[:, gi, n * 2:n * 2 + 2, :], W[nm][n].rearrange("(c p) d -> p c d", p=128), None, b_GW)
                xrr = Ring(kb, "xr", 2, [128, 2, TC + 3], F32, pes)
                xcr = Ring(kb, "xc", 2, [128, 2, TC], F32, pes)
                xcbr = Ring(kb, "xcb", 2, [128, 2, TC], BF16, pes)
                rtr = Ring(kb, "rt", 2, [128, TC], F32, pes)
                itr = Ring(kb, "it", 2, [128, TC], F32, pes)
                atr = Ring(kb, "at", 2, [128, TC], F32, pes)
                mtr = Ring(kb, "mt", 2, [128, TC], F32, pes)
                bxr = Ring(kb, "bxt", 2, [128, TC], F32, pes)
                hring = Ring(kb, "ht", 2, [128, TC], F32, pes)
                st, b_st = kb.sb("st", [128, 2], F32, pes), Buf("st")
                units = []
                for n in range(4):
                    for dirn in range(2):
                        order = list(range(NTC)) if dirn == 0 else list(range(NTC - 1, -1, -1))
                        for k_, tc in enumerate(order):
                            units.append((n, dirn, tc, k_ == 0))
                cv = {}
                gt = {}

                def conv_part(u):
                    n, dirn, tc, first = u
                    xr, b_xr = xrr.next()
                    xc, b_xc = xcr.next()
                    xcb, b_xcb = xcbr.next()
                    cv[u] = (xc, b_xc, xcb, b_xcb)
                    t0 = tc * TC
                    lo, hi = max(t0 - 1, 0), min(t0 + TC + 2, SF)
                    if lo > t0 - 1:
                        kb.op(dve, lambda: V.memset(xr[:, :, 0:1], 0.0), [], [b_xr])
                    if hi < t0 + TC + 2:
                        kb.op(dve, lambda: V.memset(xr[:, :, TC + 1:TC + 3], 0.0), [], [b_xr])
                    kb.dma(sp, xr[:, :, lo - (t0 - 1):hi - (t0 - 1)],
                           xrT[n * 256:(n + 1) * 256, lo:hi].rearrange("(c p) t -> p c t", p=128), b_xrT, b_xr)
                    for c in range(2):
                        ct = n * 2 + c
                        kb.op(dve, lambda: V.tensor_scalar(xc[:, c, :], xr[:, c, 0:TC], cw[:, 0, ct:ct + 1],
                                                           cpar[:, 0, ct:ct + 1], ALU.mult, ALU.add),
                              [b_xr, b_cw, b_cpar], [b_xc])
                        for j in range(1, 4):
                            kb.op(dve, lambda: V.scalar_tensor_tensor(xc[:, c, :], xr[:, c, j:j + TC], cw[:, j, ct:ct + 1],
                                                                      xc[:, c, :], ALU.mult, ALU.add),
                                  [b_xr, b_cw, b_xc], [b_xc])
                        kb.op(act, lambda: A.copy(xcb[:, c, :], xc[:, c, :]), [b_xc], [b_xcb])

                def gate_part(u, o):
                    n, dirn, tc, first = u
                    xc, b_xc, xcb, b_xcb = cv[u]
                    ct = n * 2 + o
                    rt, b_rt = rtr.next()
                    it, b_it = itr.next()
                    gt[(u, o)] = (rt, b_rt, it, b_it)
                    for piece in range(TC // 512):
                        sl = slice(piece * 512, (piece + 1) * 512)
                        for gate in range(2):
                            bi = (piece * 2 + gate) % 8
                            kb.op_multi(pe, [lambda c=c: T.matmul(bank[bi], GW[:, dirn * 2 + gate, n * 2 + c, o * 128:(o + 1) * 128],
                                                                  xcb[:, c, sl], start=(c == 0), stop=(c == 1)) for c in range(2)],
                                        [b_GW, b_xcb], [b_bank[bi]])
                            dst, b_dst = (rt, b_rt) if gate == 0 else (it, b_it)
                            bcol = cpar[:, 1 + dirn * 2 + gate, ct:ct + 1]
                            kb.op(act, lambda: A.activation(dst[:, sl], bank[bi], AF.Sigmoid, bias=bcol),
                                  [b_bank[bi], b_cpar], [b_dst])

                def post_part(u, o):
                    n, dirn, tc, first = u
                    xc, b_xc, xcb, b_xcb = cv[u]
                    rt, b_rt, it, b_it = gt.pop((u, o))
                    ct = n * 2 + o
                    t0 = tc * TC
                    if first and o == 0:
                        kb.op(dve, lambda: V.memset(st[:], 0.0), [], [b_st])
                    at, b_at = atr.next()
                    mt, b_mt = mtr.next()
                    bxt, b_bxt = bxr.next()
                    kb.op(act, lambda: A.activation(at[:], rt[:], AF.Exp, scale=cpar[:, 5 + dirn, ct:ct + 1]),
                          [b_rt, b_cpar], [b_at])
                    kb.op(dve, lambda: V.scalar_tensor_tensor(mt[:], at[:], -1.0, at[:], ALU.mult, ALU.mult), [b_at], [b_mt])
                    kb.op(dve, lambda: V.tensor_scalar(mt[:], mt[:], 1.0, 1e-30, ALU.add, ALU.max), [b_mt], [b_mt])
                    kb.op(act, lambda: A.activation(mt[:], mt[:], AF.Sqrt), [b_mt], [b_mt])
                    if dirn == 0 and tc == 0:
                        kb.op(dve, lambda: V.memset(mt[:, 0:1], 1.0), [], [b_mt])
                    if dirn == 1 and tc == NTC - 1:
                        kb.op(dve, lambda: V.memset(mt[:, TC - 1:TC], 1.0), [], [b_mt])
                    if dirn == 1 and tc == NTC // 2 - 1:
                        kb.op(dve, lambda: V.tensor_scalar(mt[:, TC - 1:TC], mt[:, TC - 1:TC], vP, fP, ALU.mult, ALU.add),
                              [b_mt, b_flg], [b_mt])
                    kb.op(dve, lambda: V.tensor_tensor(bxt[:], it[:], xc[:, o, :], ALU.mult), [b_it, b_xc], [b_bxt])
                    kb.op(dve, lambda: V.tensor_tensor(bxt[:], bxt[:], mt[:], ALU.mult), [b_bxt, b_mt], [b_bxt])
                    if dirn == 1 and tc >= NTC // 2:
                        kb.op(dve, lambda: V.tensor_scalar(bxt[:], bxt[:], vP, None, ALU.mult), [b_bxt, b_flg], [b_bxt])
                    ht, b_ht = hring.next()
                    if dirn == 0:
                        kb.op(dve, lambda: V.tensor_tensor_scan(ht[:], at[:], bxt[:], st[:, o:o + 1], ALU.mult, ALU.add),
                              [b_at, b_bxt, b_st], [b_ht])
                        kb.op(dve, lambda: V.tensor_copy(st[:, o:o + 1], ht[:, TC - 1:TC]), [b_ht], [b_st])
                    else:
                        kb.op(dve, lambda: V.tensor_tensor_scan(ht[:, ::-1], at[:, ::-1], bxt[:, ::-1], st[:, o:o + 1],
                                                                ALU.mult, ALU.add), [b_at, b_bxt, b_st], [b_ht])
                        kb.op(dve, lambda: V.tensor_copy(st[:, o:o + 1], ht[:, 0:1]), [b_ht], [b_st])
                    dst, b_dst = (hf, b_hf) if dirn == 0 else (hb, b_hb)
                    kb.dma(sp, dst[ct * 128:(ct + 1) * 128, t0:t0 + TC], ht[:], b_ht, b_dst)

                conv_part(units[0])
                for i_, u in enumerate(units):
                    gate_part(u, 0)
                    gate_part(u, 1)
                    if i_ + 1 < len(units):
                        conv_part(units[i_ + 1])
                    post_part(u, 0)
                    post_part(u, 1)
                    cv.pop(u)
                kb.barrier()

        if "D" in phases:
            with ExitStack() as pes:
                lq, b_lq = kb.sb("lq", [128, 4, 64], F32, pes), Buf("lq")
                for i, nm in enumerate(("lam_q1", "lam_k1", "lam_q2", "lam_k2")):
                    bcast_load(lq[:, i, :], W[nm], 64, b_lq)
                lj, b_lj = kb.sb("lj", [128, 64], F32, pes), Buf("lj")
                lam, b_lam = kb.sb("lam", [128, 4], F32, pes), Buf("lam")
                for i in range(2):
                    kb.op(dve, lambda: V.scalar_tensor_tensor(lj[:], lq[:, 2 * i, :], 1.0, lq[:, 2 * i + 1, :], ALU.mult, ALU.mult,
                                                              accum_out=lam[:, i:i + 1]), [b_lq], [b_lj, b_lam])
                kb.op(act, lambda: A.activation(lam[:, 0:2], lam[:, 0:2], AF.Exp), [b_lam], [b_lam])
                kb.op(dve, lambda: V.tensor_tensor(lam[:, 2:3], lam[:, 0:1], lam[:, 1:2], ALU.subtract), [b_lam], [b_lam])
                kb.op(dve, lambda: V.tensor_scalar(lam[:, 3:4], lam[:, 2:3], -1.0, -LAM_INIT, ALU.mult, ALU.add), [b_lam], [b_lam])
                nlam = lam[:, 3:4]
                sg, b_sg = kb.sb("sgn", [128, 1], F32, pes), Buf("sgn")
                kb.dma(sp, sg[:], W["subln_g"].rearrange("(p o) -> p o", o=1), None, b_sg)
                kb.op(dve, lambda: V.tensor_scalar(sg[:], sg[:], 1.0 - LAM_INIT, None, ALU.mult), [b_sg], [b_sg])
                rb, b_rb = kb.sb("rb", [32, 8], F32, pes), Buf("rb")
                kb.dma(sp, rb[:], W["rel_bias"], None, b_rb)
                ohb, b_ohb = kb.sb("ohbs", [32, 512], F32, pes), Buf("ohbs")
                kb.dma(sp, ohb[:], ohb_d, None, b_ohb)
                kb.op(pe, lambda: T.matmul(bank[0][0:8, :], rb[:], ohb[:], start=True, stop=True), [b_rb, b_ohb], [b_bank[0]])
                tvs, b_tvs = kb.sb("tvs", [8, 512], F32, pes), Buf("tvs")
                kb.op(dve, lambda: V.tensor_copy(tvs[:], bank[0][0:8, :]), [b_bank[0]], [b_tvs])
                kb.dma(sp, tvd, tvs[:], b_tvs, b_tvd)
                Ef, b_Ef = kb.sb("Ef", [128, 8, 3, 128], F32, pes), Buf("Ef")
                for h in range(NH):
                    for mi, m in enumerate((-1, 0, 1)):
                        src = bass.AP(tvd.tensor, h * 512 + 256 - 128 * m - 127, [[1, 128], [1, 128]])
                        kb.dma(sp, Ef[:, h, mi, :], src, b_tvd, b_Ef)
                cb, b_cb = kb.sb("cb", [128, 2, 8], F32, pes), Buf("cb")
                bcast_load(cb[:, 0, :], W["rel_bias"][15, :], 8, b_cb)
                bcast_load(cb[:, 1, :], W["rel_bias"][31, :], 8, b_cb)
                bc, b_bc = kb.sb("bc", [128, 8, 8], F32, pes), Buf("bc")
                kb.op(dve, lambda: V.memset(bc[:], 0.0), [], [b_bc])
                kb.op(dve, lambda: V.tensor_copy(bc[:, :, 0], cb[:, 0, :]), [b_cb], [b_bc])
                kb.op(dve, lambda: V.tensor_copy(bc[:, :, 2], cb[:, 1, :]), [b_cb], [b_bc])
                kb.op(dve, lambda: V.tensor_scalar(bc[:, :, 1], cb[:, 1, :], fA, None, ALU.mult), [b_cb, b_flg], [b_bc])
                kb.op(dve, lambda: V.scalar_tensor_tensor(bc[:, :, 1], cb[:, 0, :], fB, bc[:, :, 1], ALU.mult, ALU.add),
                      [b_cb, b_flg, b_bc], [b_bc])
                kb.op(dve, lambda: V.tensor_scalar(bc[:, :, 3], bc[:, :, 1], msk, None, ALU.add), [b_bc, b_flg], [b_bc])
                kb.op(dve, lambda: V.tensor_scalar(bc[:, :, 4], bc[:, :, 2], msk, None, ALU.add), [b_bc, b_flg], [b_bc])
                kb.op(dve, lambda: V.tensor_scalar(bc[:, :, 6], bc[:, :, 5], msk, None, ALU.add), [b_bc, b_flg], [b_bc])
                Gf, b_Gf = kb.sb("Gf", [128, 9, 128], F32, pes), Buf("Gf")
                Gh, b_Gh = kb.sb("Gh", [128, 9, 128], BF16, pes), Buf("Gh")
                Gl, b_Gl = kb.sb("Gl", [128, 9, 128], BF16, pes), Buf("Gl")
                tmpc, b_tmpc = kb.sb("tmpc", [128, 2], F32, pes), Buf("tmpc")

                def gidx(m, side):
                    if side == 0:
                        return 6 if m <= -2 else (7 if m >= 2 else m + 1)
                    return 7 if m <= -2 else (8 if m >= 2 else 3 + m + 1)

                kr = Ring(kb, "kTh", 2, [128, SF], BF16, pes)
                vr = Ring(kb, "vh", 2, [128, NKB, 128], BF16, pes)
                qr = Ring(kb, "qTh", 2, [128, SO], BF16, pes)
                rz, b_rz = kb.sb("rz", [128, 2, 512], F32, pes), Buf("rz")
                dd, b_dd = kb.sb("dd", [128, 2, 512], F32, pes), Buf("dd")
                sqt, b_sqt = kb.sb("sqt", [128, 512], F32, pes), Buf("sqt")
                rstd, b_rstd = kb.sb("rstd", [128, 512], F32, pes), Buf("rstd")
                orr = Ring(kb, "ot", 2, [128, 512], BF16, pes)
                ptr = Ring(kb, "ptw", 4, [128, 1024], BF16, pes)
                OB = [4, 5]
                ZB = [6, 7]
                XB = 0

                def load_head(h):
                    kTh, b_kTh = kr.next()
                    vh, b_vh = vr.next()
                    qTh, b_qTh = qr.next()
                    kb.dma(sp, kTh[:], kT[h * 128:(h + 1) * 128, :], b_kT, b_kTh)
                    kb.dma(sp, vh[:], vv[:, h * 128:(h + 1) * 128].rearrange("(a p) e -> p a e", p=128), b_vv, b_vh)
                    kb.dma(sp, qTh[:], qT[h * 128:(h + 1) * 128, :], b_qT, b_qTh)
                    return (kTh, b_kTh, vh, b_vh, qTh, b_qTh)

                pend = {"a": None, "b": None}
                nxt = load_head(0)
                convert_tables(pes)
                for h in range(NH):
                    kb.op(dve, lambda: V.tensor_scalar(tmpc[:, 0:1], cb[:, 0, h:h + 1], fB, None, ALU.mult), [b_cb, b_flg], [b_tmpc])
                    kb.op(dve, lambda: V.tensor_scalar(tmpc[:, 1:2], cb[:, 1, h:h + 1], fA, None, ALU.mult), [b_cb, b_flg], [b_tmpc])
                    for mi in range(3):
                        kb.op(dve, lambda: V.tensor_scalar(Gf[:, mi, :], Ef[:, h, mi, :], fA, tmpc[:, 0:1], ALU.mult, ALU.add),
                              [b_Ef, b_flg, b_tmpc], [b_Gf])
                        kb.op(dve, lambda: V.tensor_scalar(Gf[:, 3 + mi, :], Ef[:, h, mi, :], fB, tmpc[:, 1:2], ALU.mult, ALU.add),
                              [b_Ef, b_flg, b_tmpc], [b_Gf])
                    for ci in range(3):
                        kb.op(dve, lambda: V.tensor_scalar(Gf[:, 6 + ci, :], onesf[:], bc[:, h, ci:ci + 1], None, ALU.mult),
                              [b_onesf, b_bc], [b_Gf])
                    kb.op(dve, lambda: V.tensor_scalar(Gf[:], Gf[:], 8.0, None, ALU.mult), [b_Gf], [b_Gf])
                    kb.op(dve, lambda: V.tensor_copy(Gh[:], Gf[:]), [b_Gf], [b_Gh])
                    kb.op(dve, lambda: V.tensor_tensor(Gl[:], Gf[:], Gh[:], ALU.subtract), [b_Gf, b_Gh], [b_Gl])
                    kTh, b_kTh, vh, b_vh, qTh, b_qTh = nxt
                    if h + 1 < NH:
                        nxt = load_head(h + 1)
                    for qc in range(NQC):
                        pts = {}

                        def stage1(kbk):
                            dA = kbk - 4 * qc
                            dB = dA - OFFB
                            nearA = -1 <= dA <= 4
                            nearB = -1 <= dB <= 4
                            s_ = kbk % 2
                            fns = []
                            for c in range(2):
                                ps = slice(c * 64, (c + 1) * 64)
                                bi = 2 * s_ + c
                                fns.append(lambda ps=ps, bi=bi: T.matmul(bank[bi], kTh[ps, kbk * 128:(kbk + 1) * 128],
                                                                         qTh[ps, qc * 512:(qc + 1) * 512],
                                                                         start=True, stop=not (nearA or nearB)))
                            reads = [b_kTh, b_qTh]
                            if nearA or nearB:
                                reads += [b_anb, b_Gh, b_Gl]
                                for c in range(2):
                                    bi = 2 * s_ + c
                                    for j in range(4):
                                        gi = gidx(dA - j, 0) if nearA else gidx(dB - j, 1)
                                        fns.append(lambda bi=bi, j=j, gi=gi: T.matmul(bank[bi][:, j * 128:(j + 1) * 128], anb[:], Gh[:, gi, :],
                                                                                      start=False, stop=False))
                                        fns.append(lambda bi=bi, j=j, gi=gi: T.matmul(bank[bi][:, j * 128:(j + 1) * 128], anb[:], Gl[:, gi, :],
                                                                                      start=False, stop=(j == 3)))
                            kb.op_multi(pe, fns, reads, [b_bank[2 * s_], b_bank[2 * s_ + 1]])

                        def stage2(kbk):
                            dA = kbk - 4 * qc
                            dB = dA - OFFB
                            m2 = kbk >= OFFB
                            if (-1 <= dA <= 4) or (-1 <= dB <= 4):
                                col = 6 if m2 else 5
                            else:
                                col = 0 if dA < 5 else (1 if dB < 5 else 2)
                                if m2:
                                    col = {1: 3, 2: 4}[col]
                            s_ = kbk % 2
                            pt, b_pt = ptr.next()
                            pts[kbk] = (pt, b_pt)
                            kb.op(act, lambda: A.activation(pt[:], psA[:, s_ * 1024:(s_ + 1) * 1024], AF.Exp,
                                                            bias=bc[:, h, col:col + 1], scale=0.125),
                                  [b_bank[2 * s_], b_bank[2 * s_ + 1], b_bc], [b_pt])

                        def stage3(kbk):
                            pt, b_pt = pts.pop(kbk)
                            fns = []
                            for c in range(2):
                                fns.append(lambda c=c: T.matmul(bank[OB[c]], vh[:, kbk, :], pt[:, c * 512:(c + 1) * 512],
                                                                start=(kbk == 0), stop=(kbk == NKB - 1)))
                                fns.append(lambda c=c: T.matmul(bank[ZB[c]], onesb[:], pt[:, c * 512:(c + 1) * 512],
                                                                start=(kbk == 0), stop=(kbk == NKB - 1)))
                            kb.op_multi(pe, fns, [b_vh, b_onesb, b_pt], [b_bank[OB[0]], b_bank[OB[1]], b_bank[ZB[0]], b_bank[ZB[1]]])

                        stage1(0)
                        stage1(1)
                        for kbk in range(NKB):
                            stage2(kbk)
                            if kbk == 3 and pend["a"] is not None:
                                pend["a"](2 * (kbk % 2))
                                pend["a"] = None
                            if kbk == 7 and pend["b"] is not None:
                                pend["b"]()
                                pend["b"] = None
                            if kbk + 2 < NKB:
                                stage1(kbk + 2)
                            stage3(kbk)
                        for c in range(2):
                            kb.op(dve, lambda: V.reciprocal(rz[:, c, :], bank[ZB[c]]), [b_bank[ZB[c]]], [b_rz])
                            kb.op(dve, lambda: V.tensor_tensor(dd[:, c, :], bank[OB[c]], rz[:, c, :], ALU.mult),
                                  [b_bank[OB[c]], b_rz], [b_dd])
                        kb.op(dve, lambda: V.scalar_tensor_tensor(dd[:, 0, :], dd[:, 1, :], nlam, dd[:, 0, :], ALU.mult, ALU.add),
                              [b_dd, b_lam], [b_dd])
                        def part2a(xb):
                            kb.op(pool, lambda: P.tensor_tensor(sqt[:], dd[:, 0, :], dd[:, 0, :], ALU.mult), [b_dd], [b_sqt])
                            kb.op(pe, lambda: T.matmul(bank[xb], onesf[:], sqt[:], start=True, stop=True), [b_onesf, b_sqt], [b_bank[xb]])
                            kb.op(dve, lambda: V.tensor_scalar(rstd[:], bank[xb], 1.0 / 128, EPS, ALU.mult, ALU.add), [b_bank[xb]], [b_rstd])

                        def part2b(h=h, qc=qc):
                            kb.op(act, lambda: A.activation(rstd[:], rstd[:], AF.Ln), [b_rstd], [b_rstd])
                            kb.op(act, lambda: A.activation(rstd[:], rstd[:], AF.Exp, scale=-0.5), [b_rstd], [b_rstd])
                            ot, b_ot = orr.next()
                            kb.op(dve, lambda: V.scalar_tensor_tensor(ot[:], dd[:, 0, :], sg[:, 0:1], rstd[:], ALU.mult, ALU.mult),
                                  [b_dd, b_sg, b_rstd], [b_ot])
                            kb.dma(sp, oT[h * 128:(h + 1) * 128, qc * 512:(qc + 1) * 512], ot[:], b_ot, b_oT)

                        pend["a"], pend["b"] = part2a, part2b
                pend["a"](0)
                pend["b"]()
                kb.barrier()

        if "E" in phases:
            with ExitStack() as pes:
                WO, b_WO = kb.sb("WO", [128, 3, 8, D], BF16, pes), Buf("WO")
                for i, nm in enumerate(("w_rnn_out", "w_attn_out", "w_out")):
                    for dt in range(8):
                        kb.dma(pool, WO[:, i, dt, :], W[nm][dt * 128:(dt + 1) * 128, :], None, b_WO)
                CE = 256
                h4r = [Ring(kb, f"h4_{i}", 2, [128, 8, CE], F32, pes) for i in range(4)]
                ggr = Ring(kb, "gg", 2, [128, 8, CE], F32, pes)
                hgr = Ring(kb, "hg", 2, [128, 8, CE], BF16, pes)
                obr = Ring(kb, "ob", 2, [128, 8, CE], BF16, pes)
                grr = Ring(kb, "gr", 2, [128, 8, CE], F32, pes)
                gar = Ring(kb, "ga", 2, [128, 8, CE], F32, pes)
                m1r = Ring(kb, "m1", 2, [128, CE], F32, pes)
                m2r = Ring(kb, "m2t", 2, [128, CE], F32, pes)
                mgr = Ring(kb, "mg", 2, [128, 8, CE], BF16, pes)
                xor_ = Ring(kb, "xo", 2, [128, D], F32, pes)
                x1r = Ring(kb, "x1t", 2, [128, D], F32, pes)
                hv = lambda a: a.rearrange("(c p) t -> p c t", p=128)

                def e_load(ch):
                    t0 = ch * CE
                    h4 = [r.next() for r in h4r]
                    for i, (src, b_src, off) in enumerate(((hf, b_hf, 0), (hb, b_hb, 0), (hf, b_hf, SO), (hb, b_hb, SO))):
                        kb.dma(sp, h4[i][0][:], hv(src)[:, :, off + t0:off + t0 + CE], b_src, h4[i][1])
                    gg, b_gg = ggr.next()
                    ob, b_ob = obr.next()
                    gr, b_gr = grr.next()
                    ga, b_ga = gar.next()
                    kb.dma(sp, gg[:], hv(ggT)[:, :, t0:t0 + CE], b_ggT, b_gg)
                    kb.dma(sp, ob[:], hv(oT)[:, :, t0:t0 + CE], b_oT, b_ob)
                    kb.dma(sp, gr[:], hv(grT)[:, :, t0:t0 + CE], b_grT, b_gr)
                    kb.dma(sp, ga[:], hv(gaT)[:, :, t0:t0 + CE], b_gaT, b_ga)
                    return h4, (gg, b_gg), (ob, b_ob), (gr, b_gr), (ga, b_ga)

                NCE = SO // CE
                loaded = e_load(0)
                for ch in range(NCE):
                    t0 = ch * CE
                    h4, (gg, b_gg), (ob, b_ob), (gr, b_gr), (ga, b_ga) = loaded
                    if ch + 1 < NCE:
                        loaded = e_load(ch + 1)
                    hg, b_hg = hgr.next()
                    mg, b_mg = mgr.next()
                    a0, a1, a2, a3 = (t[0] for t in h4)
                    kb.op(dve, lambda: V.tensor_tensor(a0[:], a0[:], a1[:], ALU.add), [h4[0][1], h4[1][1]], [h4[0][1]])
                    kb.op(pool, lambda: P.tensor_tensor(a2[:], a2[:], a3[:], ALU.add), [h4[2][1], h4[3][1]], [h4[2][1]])
                    kb.op(dve, lambda: V.tensor_scalar(a0[:], a0[:], fA, None, ALU.mult), [h4[0][1], b_flg], [h4[0][1]])
                    kb.op(dve, lambda: V.scalar_tensor_tensor(a0[:], a2[:], fB, a0[:], ALU.mult, ALU.add),
                          [h4[0][1], h4[2][1], b_flg], [h4[0][1]])
                    kb.op(dve, lambda: V.tensor_tensor(hg[:], a0[:], gg[:], ALU.mult), [h4[0][1], b_gg], [b_hg])
                    for j in range(8):
                        b1, b2 = (2 * j) % 8, (2 * j + 1) % 8
                        kb.op_multi(pe, [lambda dt=dt: T.matmul(bank[b1][:, 0:CE], WO[:, 0, dt, j * 128:(j + 1) * 128], hg[:, dt, :],
                                                                start=(dt == 0), stop=(dt == 7)) for dt in range(8)],
                                    [b_WO, b_hg], [b_bank[b1]])
                        kb.op_multi(pe, [lambda dt=dt: T.matmul(bank[b2][:, 0:CE], WO[:, 1, dt, j * 128:(j + 1) * 128], ob[:, dt, :],
                                                                start=(dt == 0), stop=(dt == 7)) for dt in range(8)],
                                    [b_WO, b_ob], [b_bank[b2]])
                        m1, b_m1 = m1r.next()
                        m2t, b_m2t = m2r.next()
                        kb.op(dve, lambda: V.tensor_tensor(m1[:], bank[b1][:, 0:CE], gr[:, j, :], ALU.mult), [b_bank[b1], b_gr], [b_m1])
                        kb.op(dve, lambda: V.tensor_tensor(m2t[:], bank[b2][:, 0:CE], ga[:, j, :], ALU.mult), [b_bank[b2], b_ga], [b_m2t])
                        kb.op(pool, lambda: P.tensor_tensor(mg[:, j, :], m1[:], m2t[:], ALU.add), [b_m1, b_m2t], [b_mg])
                    for tt in range(CE // 128):
                        xt, b_xt = xor_.next()
                        kb.dma(sp, xt[:], xo[t0 + tt * 128:t0 + (tt + 1) * 128, :], None, b_xt)
                        x1t, b_x1t = x1r.next()
                        for half in range(2):
                            bi = (tt * 2 + half) % 8
                            kb.op_multi(pe, [lambda dt=dt: T.matmul(bank[bi], mg[:, dt, tt * 128:(tt + 1) * 128],
                                                                    WO[:, 2, dt, half * 512:(half + 1) * 512],
                                                                    start=(dt == 0), stop=(dt == 7)) for dt in range(8)],
                                        [b_WO, b_mg], [b_bank[bi]])
                            kb.op(dve, lambda: V.tensor_tensor(x1t[:, half * 512:(half + 1) * 512], bank[bi],
                                                               xt[:, half * 512:(half + 1) * 512], ALU.add),
                                  [b_bank[bi], b_xt], [b_x1t])
                        kb.dma(sp, x1[t0 + tt * 128:t0 + (tt + 1) * 128, :], x1t[:], b_x1t, b_x1)
                kb.barrier()

        if "F" in phases:
            with ExitStack() as pes:
                if "D" not in phases:
                    convert_tables(pes)
                g2, b_g2 = kb.sb("g2", [128, D], F32, pes), Buf("g2")
                bcast_load(g2[:], W["norm2_g"], D, b_g2)
                gf, b_gf = kb.sb("gfin", [128, D], F32, pes), Buf("gfin")
                bcast_load(gf[:], W["final_g"], D, b_gf)
                WQ, b_WQ = kb.sb("WQ", [128, 8, 2048], BF16, pes), Buf("WQ")
                for dt in range(8):
                    for g in range(2):
                        kb.dma(pool, WQ[:, dt, g * 1024:(g + 1) * 1024], W["peer_wq"][dt * 128:(dt + 1) * 128, g * 1024:(g + 1) * 1024],
                               None, b_WQ)
                kf, b_kf = kb.sb("kf", [128, 16, 128], BF16, pes), Buf("kf")
                kb.dma(pool, kf[:], W["peer_keys"].rearrange("a n d -> n a d"), None, b_kf)
                kTt, b_kTt = kb.sb("kTt", [128, 16, 128], BF16, pes), Buf("kTt")
                for hp in range(16):
                    bi = hp // 4
                    kb.op(pe, lambda: T.matmul(bank[bi][:, (hp % 4) * 128:(hp % 4 + 1) * 128], kf[:, hp, :], idb[:],
                                               start=True, stop=True), [b_kf, b_idb], [b_bank[bi]])
                    if hp % 4 == 3:
                        evac(kTt[:, bi * 4:bi * 4 + 4, :], bank[bi].rearrange("p (a b) -> p a b", a=4), [b_bank[bi]], [b_kTt])
                io16, b_io16 = kb.sb("io16", [128, 16], F32, pes), Buf("io16")
                kb.op(pool, lambda: P.iota(io16[:], [[1, 16]], base=0, channel_multiplier=0, allow_small_or_imprecise_dtypes=True),
                      [], [b_io16])
                thr16 = kb.sb("thr16", [128, 16], F32, pes)
                kb.op(dve, lambda: V.tensor_scalar(thr16[:], io16[:], 16.0, 16.0, ALU.mult, ALU.add), [b_io16], [b_io16])
                X1 = [(kb.sb(f"fx1_{i}", [128, D], F32, pes), Buf(f"fx1_{i}")) for i in range(2)]
                XN = [(kb.sb(f"fxn_{i}", [128, D], F32, pes), Buf(f"fxn_{i}")) for i in range(2)]
                IDX = [(kb.sb(f"fidx_{i}", [128, 128], U32, pes), Buf(f"fidx_{i}")) for i in range(2)]
                GW = [(kb.sb(f"fgw_{i}", [128, 8, 16], F32, pes), Buf(f"fgw_{i}")) for i in range(2)]
                junkA, b_junkA = kb.sb("fjunkA", [128, D], BF16, pes), Buf("fjunkA")
                ssA, b_ssA = kb.sb("fssA", [128, 2], F32, pes), Buf("fssA")
                junk, b_junk = kb.sb("fjunk", [128, D], F32, pes), Buf("fjunk")
                ss, b_ss = kb.sb("fss", [128, 2], F32, pes), Buf("fss")
                xnb, b_xnb = kb.sb("fxnb", [128, D], BF16, pes), Buf("fxnb")
                xT, b_xT = kb.sb("fxT", [128, 8, 128], BF16, pes), Buf("fxT")
                qTt, b_qTt = kb.sb("fqT", [128, 16, 128], BF16, pes), Buf("fqT")
                sc, b_sc = kb.sb("fsc", [128, 16, 128], F32, pes), Buf("fsc")
                sc2, b_sc2 = kb.sb("fsc2", [128, 16, 128], F32, pes), Buf("fsc2")
                svl, b_svl = kb.sb("fsv", [128, 16, 16], F32, pes), Buf("fsv")
                siu, b_siu = kb.sb("fsiu", [128, 16, 16], U32, pes), Buf("fsiu")
                sif, b_sif = kb.sb("fsif", [128, 16, 16], F32, pes), Buf("fsif")
                cand, b_cand = kb.sb("fcand", [128, 8, 256], F32, pes), Buf("fcand")
                cand2, b_cand2 = kb.sb("fcand2", [128, 8, 256], F32, pes), Buf("fcand2")
                ts, b_ts = kb.sb("fts", [128, 8, 16], F32, pes), Buf("fts")
                pu32, b_pu32 = kb.sb("fpu", [128, 8, 16], U32, pes), Buf("fpu")
                posf, b_posf = kb.sb("fposf", [128, 8, 16], F32, pes), Buf("fposf")
                pj, b_pj = kb.sb("fpj", [128, 8, 16], F32, pes), Buf("fpj")
                pi_, b_pi = kb.sb("fpi", [128, 8, 16], F32, pes), Buf("fpi")
                eq, b_eq = kb.sb("feq", [128, 8, 16, 16], F32, pes), Buf("feq")
                I1, b_I1 = kb.sb("fI1", [128, 8, 16], F32, pes), Buf("fI1")
                I2, b_I2 = kb.sb("fI2", [128, 8, 16], F32, pes), Buf("fI2")
                nmx, b_nmx = kb.sb("fnmx", [128, 8], F32, pes), Buf("fnmx")
                zs, b_zs = kb.sb("fzs", [128, 8], F32, pes), Buf("fzs")
                actt, b_actt = kb.sb("fact", [128, 128], F32, pes), Buf("fact")
                wgt, b_wgt = kb.sb("fwgt", [128, 128], F32, pes), Buf("fwgt")
                acc, b_acc = kb.sb("facc", [128, D], F32, pes), Buf("facc")
                ur = Ring(kb, "fU", 16, [128, 2 * D], BF16, pes)
                dgr = Ring(kb, "fdg", 4, [128, 128], BF16, pes)
                yt, b_yt = kb.sb("fyt", [128, D], F32, pes), Buf("fyt")
                BW = [Buf(f"wgt{i}") for i in range(32)]
                BA = [Buf(f"act{i}") for i in range(32)]
                def f_a0(tt):
                        x1t, b_x1t = X1[tt % 2]
                        xn, b_xn = XN[tt % 2]
                        idxu, b_idxu = IDX[tt % 2]
                        gw, b_gw = GW[tt % 2]
                        gwf = gw.rearrange("p h k -> p (h k)")
                        r0 = tt * 128
                        sv4 = svl.rearrange("p (h two) k -> p h two k", two=2)
                        si4 = sif.rearrange("p (h two) k -> p h two k", two=2)
                        c4 = cand.rearrange("p h (i j) -> p h i j", i=16)
                        B4 = [128, 8, 16, 16]
                        r0 = tt * 128
                        kb.dma(sp, x1t[:], x1[r0:r0 + 128, :], b_x1, b_x1t)
                        kb.op(act, lambda: A.activation(junkA[:], x1t[:], AF.Square, accum_out=ssA[:, 0:1]), [b_x1t], [b_junkA, b_ssA])
                        kb.op(act, lambda: A.activation(ssA[:, 0:1], ssA[:, 0:1], AF.Sqrt, bias=epsb[:], scale=1.0 / D), [b_ssA, b_epsb], [b_ssA])
                def f_a(tt):
                        x1t, b_x1t = X1[tt % 2]
                        xn, b_xn = XN[tt % 2]
                        idxu, b_idxu = IDX[tt % 2]
                        gw, b_gw = GW[tt % 2]
                        gwf = gw.rearrange("p h k -> p (h k)")
                        r0 = tt * 128
                        sv4 = svl.rearrange("p (h two) k -> p h two k", two=2)
                        si4 = sif.rearrange("p (h two) k -> p h two k", two=2)
                        c4 = cand.rearrange("p h (i j) -> p h i j", i=16)
                        B4 = [128, 8, 16, 16]
                        r0 = tt * 128
                        kb.op(dve, lambda: V.reciprocal(ssA[:, 0:1], ssA[:, 0:1]), [b_ssA], [b_ssA])
                        kb.op(dve, lambda: V.scalar_tensor_tensor(xn[:], x1t[:], ssA[:, 0:1], g2[:], ALU.mult, ALU.mult),
                              [b_x1t, b_ssA, b_g2], [b_xn])
                        kb.op(act, lambda: A.copy(xnb[:], xn[:]), [b_xn], [b_xnb])
                        for half in range(2):
                            bi = half
                            for q in range(4):
                                dt = half * 4 + q
                                kb.op(pe, lambda: T.matmul(bank[bi][:, q * 128:(q + 1) * 128], xnb[:, dt * 128:(dt + 1) * 128], idb[:],
                                                           start=True, stop=True), [b_xnb, b_idb], [b_bank[bi]])
                            evac(xT[:, half * 4:half * 4 + 4, :], bank[bi].rearrange("p (a b) -> p a b", a=4), [b_bank[bi]], [b_xT], force_act=True)
                        for g4 in range(4):
                            bi = 2 + g4
                            for q in range(4):
                                hp = g4 * 4 + q
                                for dt in range(8):
                                    kb.op(pe, lambda: T.matmul(bank[bi][:, q * 128:(q + 1) * 128], WQ[:, dt, hp * 128:(hp + 1) * 128],
                                                               xT[:, dt, :], start=(dt == 0), stop=(dt == 7)), [b_WQ, b_xT], [b_bank[bi]])
                            evac(qTt[:, g4 * 4:g4 * 4 + 4, :], bank[bi].rearrange("p (a b) -> p a b", a=4), [b_bank[bi]], [b_qTt], force_act=True)
                        for g4 in range(4):
                            bi = g4
                            for q in range(4):
                                hp = g4 * 4 + q
                                kb.op(pe, lambda: T.matmul(bank[bi][:, q * 128:(q + 1) * 128], qTt[:, hp, :], kTt[:, hp, :],
                                                           start=True, stop=True), [b_qTt, b_kTt], [b_bank[bi]])
                            evac(sc[:, g4 * 4:g4 * 4 + 4, :], bank[bi].rearrange("p (a b) -> p a b", a=4), [b_bank[bi]], [b_sc], force_act=True)
                def f_b(tt):
                        x1t, b_x1t = X1[tt % 2]
                        xn, b_xn = XN[tt % 2]
                        idxu, b_idxu = IDX[tt % 2]
                        gw, b_gw = GW[tt % 2]
                        gwf = gw.rearrange("p h k -> p (h k)")
                        r0 = tt * 128
                        sv4 = svl.rearrange("p (h two) k -> p h two k", two=2)
                        si4 = sif.rearrange("p (h two) k -> p h two k", two=2)
                        c4 = cand.rearrange("p h (i j) -> p h i j", i=16)
                        B4 = [128, 8, 16, 16]
                        for hp in range(16):
                            kb.op(dve, lambda: V.max(svl[:, hp, 0:8], sc[:, hp, :]), [b_sc], [b_svl])
                            kb.op(dve, lambda: V.match_replace(sc2[:, hp, :], svl[:, hp, 0:8], sc[:, hp, :], -1e30), [b_svl, b_sc], [b_sc2])
                            kb.op(dve, lambda: V.max(svl[:, hp, 8:16], sc2[:, hp, :]), [b_sc2], [b_svl])
                            kb.op(dve, lambda: V.max_index(siu[:, hp, 0:8], svl[:, hp, 0:8], sc[:, hp, :]), [b_svl, b_sc], [b_siu])
                            kb.op(dve, lambda: V.max_index(siu[:, hp, 8:16], svl[:, hp, 8:16], sc2[:, hp, :]), [b_svl, b_sc2], [b_siu])
                        kb.op(dve, lambda: V.tensor_copy(sif[:], siu[:]), [b_siu], [b_sif])
                def f_c(tt):
                        x1t, b_x1t = X1[tt % 2]
                        xn, b_xn = XN[tt % 2]
                        idxu, b_idxu = IDX[tt % 2]
                        gw, b_gw = GW[tt % 2]
                        gwf = gw.rearrange("p h k -> p (h k)")
                        r0 = tt * 128
                        sv4 = svl.rearrange("p (h two) k -> p h two k", two=2)
                        si4 = sif.rearrange("p (h two) k -> p h two k", two=2)
                        c4 = cand.rearrange("p h (i j) -> p h i j", i=16)
                        B4 = [128, 8, 16, 16]
                        sv4 = svl.rearrange("p (h two) k -> p h two k", two=2)
                        si4 = sif.rearrange("p (h two) k -> p h two k", two=2)
                        c4 = cand.rearrange("p h (i j) -> p h i j", i=16)
                        kb.op(dve, lambda: V.tensor_tensor(c4[:], sv4[:, :, 0, :].unsqueeze(3).to_broadcast(B4),
                                                           sv4[:, :, 1, :].unsqueeze(2).to_broadcast(B4), ALU.add),
                              [b_svl], [b_cand])
                        for h in range(8):
                            kb.op(dve, lambda: V.max(ts[:, h, 0:8], cand[:, h, :]), [b_cand], [b_ts])
                            kb.op(dve, lambda: V.match_replace(cand2[:, h, :], ts[:, h, 0:8], cand[:, h, :], -1e30), [b_ts, b_cand], [b_cand2])
                            kb.op(dve, lambda: V.max(ts[:, h, 8:16], cand2[:, h, :]), [b_cand2], [b_ts])
                            kb.op(dve, lambda: V.max_index(pu32[:, h, 0:8], ts[:, h, 0:8], cand[:, h, :]), [b_ts, b_cand], [b_pu32])
                            kb.op(dve, lambda: V.max_index(pu32[:, h, 8:16], ts[:, h, 8:16], cand2[:, h, :]), [b_ts, b_cand2], [b_pu32])
                        kb.op(dve, lambda: V.tensor_scalar(nmx[:], ts[:, :, 0], -1.0, None, ALU.mult), [b_ts], [b_nmx])
                        for h in range(8):
                            kb.op(act, lambda: A.activation(gw[:, h, :], ts[:, h, :], AF.Exp, bias=nmx[:, h:h + 1], accum_out=zs[:, h:h + 1]),
                                  [b_ts, b_nmx], [b_gw, b_zs])
                def f_d(tt):
                        x1t, b_x1t = X1[tt % 2]
                        xn, b_xn = XN[tt % 2]
                        idxu, b_idxu = IDX[tt % 2]
                        gw, b_gw = GW[tt % 2]
                        gwf = gw.rearrange("p h k -> p (h k)")
                        r0 = tt * 128
                        sv4 = svl.rearrange("p (h two) k -> p h two k", two=2)
                        si4 = sif.rearrange("p (h two) k -> p h two k", two=2)
                        c4 = cand.rearrange("p h (i j) -> p h i j", i=16)
                        B4 = [128, 8, 16, 16]
                        kb.op(dve, lambda: V.reciprocal(zs[:], zs[:]), [b_zs], [b_zs])
                        kb.op(dve, lambda: V.tensor_tensor(gw[:], gw[:], zs[:].unsqueeze(2).to_broadcast([128, 8, 16]), ALU.mult),
                              [b_gw, b_zs], [b_gw])
                        kb.op(dve, lambda: V.tensor_copy(posf[:], pu32[:]), [b_pu32], [b_posf])
                        B4 = [128, 8, 16, 16]
                        kb.op(dve, lambda: V.tensor_tensor(eq[:], posf[:].unsqueeze(3).to_broadcast(B4),
                                                           thr16[:].unsqueeze(1).unsqueeze(1).to_broadcast(B4), ALU.is_ge),
                              [b_posf, b_io16], [b_eq])
                        kb.op(dve, lambda: V.tensor_reduce(pi_[:], eq[:], AX.X, ALU.add), [b_eq], [b_pi])
                        kb.op(dve, lambda: V.scalar_tensor_tensor(pj[:], pi_[:], -16.0, posf[:], ALU.mult, ALU.add), [b_pi, b_posf], [b_pj])
                        for (src, b_src, two, dst, b_dst) in ((pi_, b_pi, 0, I1, b_I1), (pj, b_pj, 1, I2, b_I2)):
                            kb.op(dve, lambda: V.tensor_tensor(eq[:], src[:].unsqueeze(3).to_broadcast(B4),
                                                               io16[:].unsqueeze(1).unsqueeze(1).to_broadcast(B4), ALU.is_equal),
                                  [b_src, b_io16], [b_eq])
                            kb.op(dve, lambda: V.tensor_tensor(eq[:], eq[:], si4[:, :, two, :].unsqueeze(2).to_broadcast(B4), ALU.mult),
                                  [b_eq, b_sif], [b_eq])
                            kb.op(dve, lambda: V.tensor_reduce(dst[:], eq[:], AX.X, ALU.add), [b_eq], [b_dst])
                        kb.op(dve, lambda: V.scalar_tensor_tensor(I1[:], I1[:], 128.0, I2[:], ALU.mult, ALU.add), [b_I1, b_I2], [b_I1])
                        kb.op(dve, lambda: V.tensor_copy(idxu[:], I1[:].rearrange("p h k -> p (h k)")), [b_I1], [b_idxu])
                def f_back(tt):
                        x1t, b_x1t = X1[tt % 2]
                        xn, b_xn = XN[tt % 2]
                        idxu, b_idxu = IDX[tt % 2]
                        gw, b_gw = GW[tt % 2]
                        gwf = gw.rearrange("p h k -> p (h k)")
                        r0 = tt * 128
                        sv4 = svl.rearrange("p (h two) k -> p h two k", two=2)
                        si4 = sif.rearrange("p (h two) k -> p h two k", two=2)
                        c4 = cand.rearrange("p h (i j) -> p h i j", i=16)
                        B4 = [128, 8, 16, 16]
                        GRP = 4

                        def consume(g0, tiles):
                            gs = slice(g0, g0 + GRP)
                            kb.op(dve, lambda: V.tensor_tensor(wgt[:, gs], wgt[:, gs], gwf[:, gs], ALU.mult), [BW[g0 // GRP], b_gw], [BW[g0 // GRP]])
                            for i, hk in enumerate(range(g0, g0 + GRP)):
                                ut, b_ut = tiles[i]
                                dg, b_dg = dgr.next()
                                kb.op(act, lambda: A.activation(dg[:], idf[:], AF.Copy, scale=wgt[:, hk:hk + 1]), [b_idf, BW[g0 // GRP]], [b_dg])
                                kb.op_multi(pe, [
                                    lambda: T.matmul(bank[6], dg[:], ut[:, D:D + 512], start=(hk == 0), stop=(hk == 127)),
                                    lambda: T.matmul(bank[7], dg[:], ut[:, D + 512:2 * D], start=(hk == 0), stop=(hk == 127)),
                                ], [b_dg, b_ut], [b_bank[6], b_bank[7]])

                        prev = None
                        for g0 in range(0, 128, GRP):
                            if nxt_stage.get(g0) is not None and tt + 1 < NTILES:
                                nxt_stage[g0](tt + 1)
                            tiles = []
                            for hk in range(g0, g0 + GRP):
                                ut, b_ut = ur.next()
                                tiles.append((ut, b_ut))
                                kb.dma(pool, ut[:], puv, b_puv, b_ut, indirect=(idxu[:, hk:hk + 1], b_idxu))
                                kb.op(dve, lambda: V.scalar_tensor_tensor(junk[:], ut[:, 0:D], 1.0, xn[:], ALU.mult, ALU.mult,
                                                                          accum_out=actt[:, hk:hk + 1]), [b_ut, b_xn], [b_junk, BA[g0 // GRP]])
                            gs = slice(g0, g0 + GRP)
                            kb.op(act, lambda: A.activation(wgt[:, gs], actt[:, gs], AF.Gelu_apprx_tanh), [BA[g0 // GRP]], [BW[g0 // GRP]])
                            if prev is not None:
                                consume(*prev)
                            prev = (g0, tiles)
                        consume(*prev)
                        for half in range(2):
                            kb.op(dve, lambda: V.tensor_tensor(acc[:, half * 512:(half + 1) * 512], bank[6 + half],
                                                               x1t[:, half * 512:(half + 1) * 512], ALU.add),
                                  [b_bank[6 + half], b_x1t], [b_acc])
                        kb.op(act, lambda: A.activation(junk[:], acc[:], AF.Square, accum_out=ss[:, 1:2]), [b_acc], [b_junk, b_ss])
                        kb.op(act, lambda: A.activation(ss[:, 1:2], ss[:, 1:2], AF.Sqrt, bias=epsb[:], scale=1.0 / D), [b_ss, b_epsb], [b_ss])
                        kb.op(dve, lambda: V.reciprocal(ss[:, 1:2], ss[:, 1:2]), [b_ss], [b_ss])
                        kb.op(dve, lambda: V.scalar_tensor_tensor(yt[:], acc[:], ss[:, 1:2], gf[:], ALU.mult, ALU.mult),
                              [b_acc, b_ss, b_gf], [b_yt])
                        kb.dma(sp, y[r0:r0 + 128, :], yt[:], b_yt, b_y)
                NTILES = SO // 128
                nxt_stage = {0: f_a0, 8: f_a, 40: f_b, 72: f_c, 104: f_d}
                for st in (f_a0, f_a, f_b, f_c, f_d):
                    st(0)
                for tt in range(NTILES):
                    f_back(tt)
                kb.barrier()
        kb.barrier()
        build.stats = (kb.ninst, kb.nwait, kb.nsem)
    return nc


def _rel_bucket_np(rel):
    nb = 16
    ret = np.where(rel > 0, nb, 0).astype(np.int32)
    n = np.abs(rel)
    max_exact = 8
    nf = np.maximum(n, 1).astype(np.float32)
    large = max_exact + (np.log(nf / max_exact) / np.float32(math.log(128 / max_exact)) * (nb - max_exact)).astype(np.int32)
    large = np.minimum(large, nb - 1)
    return ret + np.where(n < max_exact, n, large)


def host_consts():
    r = np.arange(512)
    rel = 256 - r
    b = _rel_bucket_np(rel)
    ohb = np.zeros((32, 512), np.float32)
    ohb[b, r] = 1.0
    return {
        "ident": np.eye(128, dtype=np.float32),
        "antiid": np.ascontiguousarray(np.eye(128, dtype=np.float32)[::-1]),
        "ohb": ohb,
    }


def squeeze_weights(inputs):
    out = {}
    for n, s in WNAMES:
        a = np.asarray(inputs[n], dtype=np.float32)
        out[n] = np.ascontiguousarray(a.reshape(s))
    return out


def make_in_maps(x_prompt, x_sample, weights, SO):
    consts = host_consts()
    maps = []

    def mk(xf, xo, fl):
        m = {"xf": np.ascontiguousarray(xf, dtype=np.float32), "xo": np.ascontiguousarray(xo, dtype=np.float32),
             "flags": np.tile(np.asarray(fl, np.float32)[None, :], (128, 1))}
        m.update(consts)
        m.update(weights)
        return m

    for b in range(x_prompt.shape[0]):
        xs = x_prompt[b]
        xf = np.concatenate([xs, np.zeros_like(xs)], axis=0)
        maps.append(mk(xf, xs, [1, 0, 1, 0]))
    for b in range(x_sample.shape[0]):
        xs = x_sample[b]
        maps.append(mk(xs, xs[:SO], [1, 0, 0, 0]))
        maps.append(mk(xs, xs[SO:], [0, 1, 0, 0]))
    return maps


_NC_CACHE = {}


def kernel(**inputs):
    x_prompt = np.asarray(inputs["x_prompt"], np.float32)
    x_sample = np.asarray(inputs["x_sample"], np.float32)
    SO = x_prompt.shape[1]
    assert x_sample.shape[1] == 2 * SO
    weights = squeeze_weights(inputs)
    maps = make_in_maps(x_prompt, x_sample, weights, SO)
    n = len(maps)
    if SO not in _NC_CACHE:
        _NC_CACHE[SO] = build(SO)
    nc = _NC_CACHE[SO]
    res = run_bass_kernel_spmd(nc, maps, core_ids=list(range(n)))
    outs = [np.asarray(r["y"], np.float32) for r in res.results]
    nb = x_prompt.shape[0]
    y_prompt = np.stack(outs[:nb], axis=0)
    y_sample = np.stack([np.concatenate([outs[nb + 2 * b], outs[nb + 2 * b + 1]], axis=0) for b in range(x_sample.shape[0])], axis=0)
    return (y_prompt, y_sample)
```
